# Optimizing a Trainium2 kernel written in Bass

```python
import jax
import jax.numpy as jnp
from jax import lax
import numpy as np

D_MODEL = 2048
BATCH = 16
SEQ = 2048
DEPTH = 4

GRID_W = 64
CTX_LEN = 256
HEAD_DIM = 128
NA_HEADS = D_MODEL // (4 * HEAD_DIM)
NA_W = NA_HEADS * HEAD_DIM
NA_WIN_R = 8
NA_WIN_C = 16
NA_QCB = 16
NA_KCB = 32
DF_HEADS = D_MODEL // (4 * HEAD_DIM)
DF_DIM = HEAD_DIM // 2
DF_W = DF_HEADS * HEAD_DIM
DF_QBLK = 128
RG_WIDTH = D_MODEL // 2
RG_BLOCKS = 8
RG_BW = RG_WIDTH // RG_BLOCKS
RG_CONV = 4
RG_C = 8.0
N_BRANCH = 3
MIX_W = NA_W + DF_W + RG_WIDTH
MIX_SIZES = (NA_W, NA_W, DF_W, DF_W, RG_WIDTH, NA_W, DF_W, RG_WIDTH)
KV_COLS = 2 * NA_W + 2 * DF_W + RG_WIDTH
MIX_COLS = KV_COLS + NA_W + DF_W + RG_WIDTH
IN_COLS = MIX_COLS + N_BRANCH * D_MODEL
N_GROUPS = 4
EXPERTS_PER_GROUP = 8
N_EXPERTS = N_GROUPS * EXPERTS_PER_GROUP
TOP_K = 2
D_EXPERT = D_MODEL // 4
MOE_BLK = 128
ROPE_BASE = 10000.0
EPS = 1e-6
NEG = -1e30

kernel_name = 'hybrid_na_diff_rglru_hmoe_dit'


def rmsnorm(x, g):
    xf = x.astype(jnp.float32)
    y = xf * lax.rsqrt(jnp.mean(xf * xf, axis=-1, keepdims=True) + EPS)
    return (y * g.astype(jnp.float32)).astype(x.dtype)


def _split(z, sizes):
    return jnp.split(z, np.cumsum(sizes)[:-1].tolist(), axis=-1)


def _heads(t, h):
    return t.reshape(*t.shape[:-1], h, t.shape[-1] // h)


def _df_heads(t, g):
    return rmsnorm(t.reshape(*t.shape[:-1], DF_HEADS, 2, DF_DIM), g)


def axial_rope_tables(n_tok):
    t = jnp.arange(n_tok, dtype=jnp.int32)
    pos = jnp.stack([t // GRID_W, t % GRID_W], axis=0).astype(jnp.float32)
    n_freq = DF_DIM // 4
    inv = ROPE_BASE ** (-jnp.arange(n_freq, dtype=jnp.float32) / n_freq)
    ang = pos[:, :, None] * inv
    return jnp.cos(ang)[:, :, None, None, :], jnp.sin(ang)[:, :, None, None, :]


def axial_rope(x, cos, sin):
    half = x.shape[-1] // 2

    def rot(u, cs, sn):
        u1, u2 = jnp.split(u, 2, axis=-1)
        return jnp.concatenate([u1 * cs - u2 * sn, u1 * sn + u2 * cs], axis=-1)

    out = jnp.concatenate([rot(x[..., :half], cos[0], sin[0]), rot(x[..., half:], cos[1], sin[1])], axis=-1)
    return out.astype(x.dtype)


def neighbourhood_attention(q, k, v, kc, vc, rpb):
    B, S, H, Dh = q.shape
    R = S // GRID_W
    kr = min(NA_WIN_R, R)
    ncb = GRID_W // NA_QCB
    scale = Dh ** -0.5
    rows = np.arange(R)
    row_start = np.clip(rows - NA_WIN_R // 2, 0, R - kr)
    dr = row_start[:, None] + np.arange(kr)[None, :] - rows[:, None] + (NA_WIN_R - 1)
    qcols = np.arange(GRID_W).reshape(ncb, NA_QCB)
    kcol_start = np.clip(np.arange(ncb) * NA_QCB - (NA_KCB - NA_QCB) // 2, 0, GRID_W - NA_KCB)
    key_cols = kcol_start[:, None] + np.arange(NA_KCB)[None, :]
    qwin = np.clip(qcols - NA_WIN_C // 2, 0, GRID_W - NA_WIN_C)[..., None]
    kcb = key_cols[:, None, :]
    col_ok = (kcb >= qwin) & (kcb < qwin + NA_WIN_C)
    dc = np.clip(kcb - qcols[..., None], 1 - NA_WIN_C, NA_WIN_C - 1) + (NA_WIN_C - 1)
    bias_cols = jnp.where(col_ok, rpb[:, :, dc], NEG)
    kg = k.reshape(B, R, GRID_W, H, Dh)
    vg = v.reshape(B, R, GRID_W, H, Dh)
    q_rows = q.reshape(B, R, GRID_W, H, Dh).swapaxes(0, 1)
    n_loc = kr * NA_KCB

    def row_step(args):
        q_r, rs, dr_r = args
        kb = lax.dynamic_slice_in_dim(kg, rs, kr, axis=1)[:, :, key_cols]
        vb = lax.dynamic_slice_in_dim(vg, rs, kr, axis=1)[:, :, key_cols]
        qb = q_r.reshape(B, ncb, NA_QCB, H, Dh)
        bias = bias_cols[:, dr_r].transpose(2, 0, 3, 1, 4)
        s_loc = jnp.einsum('bnqhd,binkhd->bnhqik', qb, kb).astype(jnp.float32) * scale + bias
        s_ctx = jnp.einsum('bnqhd,bchd->bnhqc', qb, kc).astype(jnp.float32) * scale
        p = jax.nn.softmax(jnp.concatenate([s_loc.reshape(B, ncb, H, NA_QCB, n_loc), s_ctx], axis=-1), axis=-1)
        p_loc = p[..., :n_loc].reshape(B, ncb, H, NA_QCB, kr, NA_KCB).astype(v.dtype)
        p_ctx = p[..., n_loc:].astype(v.dtype)
        o = jnp.einsum('bnhqik,binkhd->bnqhd', p_loc, vb) + jnp.einsum('bnhqc,bchd->bnqhd', p_ctx, vc)
        return o.reshape(B, GRID_W, H, Dh)

    out = lax.map(row_step, (q_rows, jnp.asarray(row_start, jnp.int32), jnp.asarray(dr, jnp.int32)))
    return out.swapaxes(0, 1).reshape(B, S, H * Dh)


def context_attention(q, k, v):
    s = jnp.einsum('bqhd,bkhd->bhqk', q, k).astype(jnp.float32) * q.shape[-1] ** -0.5
    p = jax.nn.softmax(s, axis=-1).astype(v.dtype)
    return jnp.einsum('bhqk,bkhd->bqhd', p, v).reshape(q.shape[0], q.shape[1], -1)


def diff_weights(s, lam, dtype):
    p = jax.nn.softmax(s, axis=-1)
    return (p[:, :, 0] - lam * p[:, :, 1]).astype(dtype)


def diff_attention_latent(q, k, v, kc, vc, lam):
    B, S, H, _, dd = q.shape
    scale = dd ** -0.5
    q_blocks = q.reshape(B, S // DF_QBLK, DF_QBLK, H, 2, dd).swapaxes(0, 1)

    def blk(qb):
        s = jnp.concatenate([jnp.einsum('bqhmd,bkhmd->bhmqk', qb, k),
                             jnp.einsum('bqhmd,bkhmd->bhmqk', qb, kc)], axis=-1).astype(jnp.float32) * scale
        w = diff_weights(s, lam, v.dtype)
        return jnp.einsum('bhqk,bkhe->bqhe', w[..., :S], v) + jnp.einsum('bhqk,bkhe->bqhe', w[..., S:], vc)

    o = lax.map(blk, q_blocks)
    return o.swapaxes(0, 1).reshape(B, S, H, -1)


def diff_attention_context(q, k, v, lam):
    s = jnp.einsum('bqhmd,bkhmd->bhmqk', q, k).astype(jnp.float32) * q.shape[-1] ** -0.5
    return jnp.einsum('bhqk,bkhe->bqhe', diff_weights(s, lam, v.dtype), v)


def diff_output(o, g, lam_init):
    return (rmsnorm(o, g) * (1.0 - lam_init)).reshape(o.shape[0], o.shape[1], -1)


def short_conv(u, w, b):
    out = lax.conv_general_dilated(u, w[:, None, :].astype(u.dtype), (1,),
                                   [(RG_CONV // 2, RG_CONV - 1 - RG_CONV // 2)],
                                   dimension_numbers=('NWC', 'WIO', 'NWC'), feature_group_count=u.shape[-1])
    return out + b


def blockdiag(u, w):
    nb, bw, _ = w.shape
    return jnp.einsum('blnd,nde->blne', u.reshape(*u.shape[:-1], nb, bw), w).reshape(u.shape)


def rg_coeffs(u, w_a, b_a, w_x, b_x, lam):
    r = jax.nn.sigmoid(blockdiag(u, w_a) + b_a).astype(jnp.float32)
    i = jax.nn.sigmoid(blockdiag(u, w_x) + b_x)
    log_a = -RG_C * r * jax.nn.softplus(-lam.astype(jnp.float32))
    a = jnp.exp(log_a)
    mult = jnp.sqrt(-jnp.expm1(2.0 * log_a))
    return a, mult * (i * u).astype(jnp.float32)


def _scan_combine(left, right):
    return left[0] * right[0], right[0] * left[1] + right[1]


def linear_scan(a, b, h0, reverse):
    if reverse:
        a, b = jnp.flip(a, 1), jnp.flip(b, 1)
    if h0 is not None:
        b = b.at[:, 0].add(a[:, 0] * h0)
    _, h = lax.associative_scan(_scan_combine, (a, b), axis=1)
    return jnp.flip(h, 1) if reverse else h


def branch_merge(ys, zg, wb, wo):
    g_a, g_b, g_c = jnp.split(jax.nn.sigmoid(zg), N_BRANCH, axis=-1)
    wb_a, wb_b, wb_c = jnp.split(wb, [NA_W, NA_W + DF_W], axis=0)
    m = g_a * (ys[0] @ wb_a) + g_b * (ys[1] @ wb_b) + g_c * (ys[2] @ wb_c)
    return m @ wo


def routed_experts(t, eid, wt, w1, w3, w2):
    n_tok, d = t.shape
    m = eid.shape[0]
    tok = jnp.arange(m, dtype=jnp.int32) // TOP_K
    order = jnp.argsort(eid, stable=True)
    se, stok, sw = eid[order], tok[order], wt[order]
    counts = jnp.bincount(eid, length=N_EXPERTS)
    starts = jnp.cumsum(counts) - counts
    pcounts = (counts + MOE_BLK - 1) // MOE_BLK * MOE_BLK
    pends = jnp.cumsum(pcounts)
    dest = (pends - pcounts)[se] + jnp.arange(m, dtype=jnp.int32) - starts[se]
    n_blk = (m + N_EXPERTS * (MOE_BLK - 1) + MOE_BLK - 1) // MOE_BLK
    n_pad = n_blk * MOE_BLK
    buf_tok = jnp.full((n_pad,), n_tok, jnp.int32).at[dest].set(stok)
    buf_w = jnp.zeros((n_pad,), t.dtype).at[dest].set(sw.astype(t.dtype))
    blk_e = jnp.minimum(jnp.searchsorted(pends, jnp.arange(n_blk) * MOE_BLK, side='right'), N_EXPERTS - 1)
    t_pad = jnp.concatenate([t, jnp.zeros((1, d), t.dtype)], axis=0)

    def block(args):
        idx, wb, e = args
        xb = t_pad[idx]
        hid = jax.nn.silu(xb @ w1[e]) * (xb @ w3[e])
        return (hid @ w2[e]) * wb[:, None]

    y = lax.map(block, (buf_tok.reshape(n_blk, MOE_BLK), buf_w.reshape(n_blk, MOE_BLK), blk_e))
    out = jnp.zeros((n_tok + 1, d), y.dtype).at[buf_tok].add(y.reshape(n_pad, d))
    return out[:n_tok]


def hierarchical_moe(t, w_rg, b_rg, w_re, b_re, w1, w3, w2):
    n = t.shape[0]
    g_logits = (t @ w_rg).astype(jnp.float32) + b_rg.astype(jnp.float32)
    g_prob = jax.nn.softmax(g_logits, axis=-1)
    grp = jnp.argmax(g_logits, axis=-1)
    p_grp = jnp.take_along_axis(g_prob, grp[:, None], axis=-1)
    e_logits = ((t @ w_re).astype(jnp.float32) + b_re.astype(jnp.float32)).reshape(n, N_GROUPS, EXPERTS_PER_GROUP)
    e_in = jnp.take_along_axis(e_logits, grp[:, None, None], axis=1)[:, 0]
    top_v, top_i = lax.top_k(e_in, TOP_K)
    w = jax.nn.softmax(top_v, axis=-1) * p_grp
    eid = (grp[:, None] * EXPERTS_PER_GROUP + top_i).astype(jnp.int32)
    return routed_experts(t, eid.reshape(-1), w.reshape(-1), w1, w3, w2)


def setup_inputs(seed: int = 0) -> dict:
    key = jax.random.key(seed)
    ks = iter(jax.random.split(key, 40))
    f32 = jnp.float32
    L, D = DEPTH, D_MODEL

    def nrm(shape, scale):
        return jax.random.normal(next(ks), shape, f32) * scale

    def gain(shape):
        return 1.0 + nrm(shape, 0.02)

    inp = {}
    inp['x'] = nrm((BATCH, SEQ, D), 1.0)
    inp['c'] = nrm((BATCH, D), 1.0)
    inp['ctx'] = nrm((BATCH, CTX_LEN, D), 1.0)
    inp['c_ctx'] = nrm((D,), 1.0)
    inp['w_mod'] = nrm((L, D, 6 * D), 0.5 * D ** -0.5)
    inp['b_mod'] = nrm((L, 6 * D), 0.01)
    inp['norm1_g'] = gain((L, D))
    inp['norm2_g'] = gain((L, D))
    inp['w_in'] = nrm((L, D, IN_COLS), D ** -0.5)
    inp['na_q_g'] = gain((L, HEAD_DIM))
    inp['na_k_g'] = gain((L, HEAD_DIM))
    inp['na_rpb'] = nrm((L, NA_HEADS, 2 * NA_WIN_R - 1, 2 * NA_WIN_C - 1), 0.1)
    inp['df_q_g'] = gain((L, DF_DIM))
    inp['df_k_g'] = gain((L, DF_DIM))
    inp['df_lam'] = nrm((L, 4, DF_DIM), 0.1)
    inp['df_sub_g'] = gain((L, 2 * DF_DIM))
    inp['rg_conv_w'] = nrm((L, RG_CONV, RG_WIDTH), RG_CONV ** -0.5)
    inp['rg_conv_b'] = nrm((L, RG_WIDTH), 0.01)
    inp['rg_w_a'] = nrm((L, 2, RG_BLOCKS, RG_BW, RG_BW), RG_BW ** -0.5)
    inp['rg_b_a'] = nrm((L, 2, RG_WIDTH), 0.01)
    inp['rg_w_x'] = nrm((L, 2, RG_BLOCKS, RG_BW, RG_BW), RG_BW ** -0.5)
    inp['rg_b_x'] = nrm((L, 2, RG_WIDTH), 0.01)
    a0 = jax.random.uniform(next(ks), (L, 2, RG_WIDTH), f32, 0.9, 0.999)
    root = a0 ** (1.0 / RG_C)
    inp['rg_lam'] = jnp.log(root) - jnp.log1p(-root)
    inp['w_branch'] = jnp.concatenate([nrm((L, NA_W, D), NA_W ** -0.5),
                                       nrm((L, DF_W, D), DF_W ** -0.5),
                                       nrm((L, RG_WIDTH, D), RG_WIDTH ** -0.5)], axis=1)
    inp['w_out'] = nrm((L, D, D), D ** -0.5)
    inp['w_router_g'] = nrm((L, D, N_GROUPS), D ** -0.5)
    inp['b_router_g'] = nrm((L, N_GROUPS), 0.01)
    inp['w_router_e'] = nrm((L, D, N_EXPERTS), D ** -0.5)
    inp['b_router_e'] = nrm((L, N_EXPERTS), 0.01)
    inp['w1'] = nrm((L, N_EXPERTS, D, D_EXPERT), D ** -0.5)
    inp['w3'] = nrm((L, N_EXPERTS, D, D_EXPERT), D ** -0.5)
    inp['w2'] = nrm((L, N_EXPERTS, D_EXPERT, D), D_EXPERT ** -0.5)
    return inp


def reference(x, c, ctx, c_ctx, w_mod, b_mod, norm1_g, norm2_g, w_in, na_q_g, na_k_g, na_rpb,
              df_q_g, df_k_g, df_lam, df_sub_g, rg_conv_w, rg_conv_b, rg_w_a, rg_b_a, rg_w_x, rg_b_x,
              rg_lam, w_branch, w_out, w_router_g, b_router_g, w_router_e, b_router_e, w1, w3, w2):
    B, S, D = x.shape
    C = ctx.shape[1]
    rope_cos, rope_sin = axial_rope_tables(S)
    silu_c = jax.nn.silu(c)
    silu_cc = jax.nn.silu(c_ctx)
    xc = ctx
    for l in range(DEPTH):
        need_ctx = l < DEPTH - 1
        lam_init = 0.8 - 0.6 * float(np.exp(-0.3 * l))
        mod = silu_c @ w_mod[l] + b_mod[l]
        sh1, sc1, gt1, sh2, sc2, gt2 = [m[:, None, :] for m in jnp.split(mod, 6, axis=-1)]
        n_cm = 6 if need_ctx else 2
        modc = jnp.split(silu_cc @ w_mod[l][:, :n_cm * D] + b_mod[l][:n_cm * D], n_cm)

        hx = rmsnorm(x, norm1_g[l]) * (1 + sc1) + sh1
        hc = rmsnorm(xc, norm1_g[l]) * (1 + modc[1]) + modc[0]
        n_cc = len(MIX_SIZES) if need_ctx else 5
        zx = _split(hx @ w_in[l][:, :MIX_COLS], MIX_SIZES)
        zc = _split(hc @ w_in[l][:, :sum(MIX_SIZES[:n_cc])], MIX_SIZES[:n_cc])

        na_kx = rmsnorm(_heads(zx[0], NA_HEADS), na_k_g[l])
        na_vx = _heads(zx[1], NA_HEADS)
        na_qx = rmsnorm(_heads(zx[5], NA_HEADS), na_q_g[l])
        na_kc = rmsnorm(_heads(zc[0], NA_HEADS), na_k_g[l])
        na_vc = _heads(zc[1], NA_HEADS)
        y_a = neighbourhood_attention(na_qx, na_kx, na_vx, na_kc, na_vc, na_rpb[l])

        lp = df_lam[l].astype(jnp.float32)
        lam = jnp.exp(jnp.sum(lp[0] * lp[1])) - jnp.exp(jnp.sum(lp[2] * lp[3])) + lam_init
        df_qx = axial_rope(_df_heads(zx[6], df_q_g[l]), rope_cos, rope_sin)
        df_kx = axial_rope(_df_heads(zx[2], df_k_g[l]), rope_cos, rope_sin)
        df_vx = _heads(zx[3], DF_HEADS)
        df_kc = _df_heads(zc[2], df_k_g[l])
        df_vc = _heads(zc[3], DF_HEADS)
        y_b = diff_output(diff_attention_latent(df_qx, df_kx, df_vx, df_kc, df_vc, lam), df_sub_g[l], lam_init)

        u_c = short_conv(zc[4], rg_conv_w[l], rg_conv_b[l])
        u_x = short_conv(zx[4], rg_conv_w[l], rg_conv_b[l])
        h_ctx = []
        h_lat = []
        for d, rev in ((0, False), (1, True)):
            a_c, b_c = rg_coeffs(u_c, rg_w_a[l, d], rg_b_a[l, d], rg_w_x[l, d], rg_b_x[l, d], rg_lam[l, d])
            hc_d = linear_scan(a_c, b_c, None, rev)
            h0 = hc_d[:, 0] if rev else hc_d[:, -1]
            a_x, b_x = rg_coeffs(u_x, rg_w_a[l, d], rg_b_a[l, d], rg_w_x[l, d], rg_b_x[l, d], rg_lam[l, d])
            h_ctx.append(hc_d)
            h_lat.append(linear_scan(a_x, b_x, h0, rev))
        y_c = (h_lat[0] + h_lat[1]).astype(zx[7].dtype) * jax.nn.gelu(zx[7], approximate=True)

        x_mid = x + gt1 * branch_merge((y_a, y_b, y_c), hx @ w_in[l][:, MIX_COLS:], w_branch[l], w_out[l])
        if need_ctx:
            na_qc = rmsnorm(_heads(zc[5], NA_HEADS), na_q_g[l])
            y_ac = context_attention(na_qc, na_kc, na_vc)
            df_qc = _df_heads(zc[6], df_q_g[l])
            y_bc = diff_output(diff_attention_context(df_qc, df_kc, df_vc, lam), df_sub_g[l], lam_init)
            y_cc = (h_ctx[0] + h_ctx[1]).astype(zc[7].dtype) * jax.nn.gelu(zc[7], approximate=True)
            xc_mid = xc + modc[2] * branch_merge((y_ac, y_bc, y_cc), hc @ w_in[l][:, MIX_COLS:], w_branch[l], w_out[l])
        x = x_mid

        hx2 = rmsnorm(x, norm2_g[l]) * (1 + sc2) + sh2
        moe_p = (w_router_g[l], b_router_g[l], w_router_e[l], b_router_e[l], w1[l], w3[l], w2[l])
        if need_ctx:
            hc2 = rmsnorm(xc_mid, norm2_g[l]) * (1 + modc[4]) + modc[3]
            toks = jnp.concatenate([hc2.reshape(B * C, D), hx2.reshape(B * S, D)], axis=0)
            f = hierarchical_moe(toks, *moe_p)
            xc = xc_mid + modc[5] * f[:B * C].reshape(B, C, D)
            fx = f[B * C:]
        else:
            fx = hierarchical_moe(hx2.reshape(B * S, D), *moe_p)
        x = x + gt2 * fx.reshape(B, S, D)
    return x
```

```python
import numpy as np
import concourse.bass as bass
import concourse.mybir as mybir
from concourse.bass_utils import run_bass_kernel_spmd
from contextlib import ExitStack

F32 = mybir.dt.float32
BF16 = mybir.dt.bfloat16
AF = mybir.ActivationFunctionType
ALU = mybir.AluOpType

L_ = 4
D = 2048
SEQ = 2048
CTX = 256
T = SEQ + CTX
NT = T // 128
NB = 2
EPS = 1e-6
NEGB = -30000.0
MIXC = 5120
INC = 11264
NPV = 96

G = {}
DEBUG = {}
PEW = "dve"

ENGS = ("pe", "act", "dve", "pool", "sp")
NSLOT = {"sp": 12, "pool": 8}


class Buf:
    __slots__ = ("w", "r", "excl")

    def __init__(self, excl=False):
        self.w = None
        self.r = {}
        self.excl = excl


def bufs(n):
    return [Buf() for _ in range(n)]


class Prog:
    def __init__(self, nc):
        self.nc = nc
        self.es = ExitStack()
        self.ops = {e: [] for e in ENGS}
        self.cnt = {e: 0 for e in ENGS}
        self.seen = {e: {} for e in ENGS}
        self.dcnt = {q: 0 for q in NSLOT}
        self.sems = {}
        for e in ("pe", "act", "dve", "pool"):
            self.sems[e] = self.es.enter_context(nc.semaphore("p_" + e))
        for q, n in NSLOT.items():
            for i in range(n):
                self.sems[(q, i)] = self.es.enter_context(nc.semaphore("d_%s%d" % (q, i)))

    def sb(self, name, shape, dtype=F32):
        return self.es.enter_context(self.nc.sbuf_tensor(name, list(shape), dtype))

    def ps(self, name, shape, dtype=F32):
        return self.es.enter_context(self.nc.psum_tensor(name, list(shape), dtype))

    def _waits(self, eng, reads, writes, extra=()):
        deps = {}

        def add(k, v):
            if deps.get(k, 0) < v:
                deps[k] = v
        for b in reads:
            if b.w is not None:
                add(*b.w)
            if b.excl:
                for k, v in b.r.items():
                    add(k, v)
        for b in writes:
            if b.w is not None:
                add(*b.w)
            for k, v in b.r.items():
                add(k, v)
        for k, v in extra:
            add(k, v)
        out = []
        seen = self.seen[eng]
        for k, v in deps.items():
            if k == eng and eng == "pe":
                continue
            if seen.get(k, 0) >= v:
                continue
            seen[k] = v
            out.append((k, v))
        return out

    def _mark(self, tok, reads, writes):
        k, v = tok
        for b in reads:
            if b.r.get(k, 0) < v:
                b.r[k] = v
        for b in writes:
            b.w = tok
            b.r = {}

    def op(self, eng, fn, reads=(), writes=()):
        waits = self._waits(eng, reads, writes)
        self.cnt[eng] += 1
        tok = (eng, self.cnt[eng])
        self.ops[eng].append((waits, fn, eng, 1))
        self._mark(tok, reads, writes)
        return tok

    def dma(self, q, fn, reads=(), writes=()):
        n = self.dcnt[q]
        ns = NSLOT[q]
        slot = (q, n % ns)
        prev = 16 * (n // ns)
        extra = [(slot, prev)] if prev > 0 else []
        waits = self._waits(q, reads, writes, extra)
        self.dcnt[q] += 1
        tok = (slot, prev + 16)
        self.ops[q].append((waits, fn, slot, 16))
        self._mark(tok, reads, writes)
        return tok

    def all_tokens(self):
        toks = [(e, self.cnt[e]) for e in ("pe", "act", "dve", "pool") if self.cnt[e] > 0]
        for q, ns in NSLOT.items():
            n = self.dcnt[q]
            for s in range(ns):
                if n > s:
                    toks.append(((q, s), 16 * ((n - 1 - s) // ns + 1)))
        return toks

    def fence(self):
        toks = self.all_tokens()
        for e in ENGS:
            waits = self._waits(e, (), (), toks)
            if waits:
                self.ops[e].append((waits, None, None, 0))

    def emit(self):
        nc = self.nc
        with nc.Block() as block:
            def run(name):
                def body(e):
                    for waits, fn, semk, inc in self.ops[name]:
                        for k, v in waits:
                            e.wait_ge(self.sems[k], v)
                        if fn is not None:
                            fn(e).then_inc(self.sems[semk], inc)
                return body
            block.tensor(run("pe"))
            block.scalar(run("act"))
            block.vector(run("dve"))
            block.gpsimd(run("pool"))
            block.sync(run("sp"))
        self.es.close()

    def mm(self, out, lhsT, rhs, start, stop, R, W):
        return self.op("pe", lambda e: e.matmul(out, lhsT, rhs, start=start, stop=stop), R, W)

    def tr(self, out, in_, ident, R, W):
        return self.op("pe", lambda e: e.transpose(out, in_, ident), R, W)

    def act(self, out, in_, func, R, W, scale=1.0, bias=0.0, accum=None):
        assert not isinstance(bias, float) or bias == 0.0, "float bias is mis-encoded as a pointer; pass an AP"
        if accum is None:
            return self.op("act", lambda e: e.activation(out=out, in_=in_, func=func, bias=bias, scale=scale), R, W)
        return self.op("act", lambda e: e.activation(out=out, in_=in_, func=func, bias=bias, scale=scale,
                                                     accum_out=accum), R, W)

    def ts(self, eng, out, in0, s1, s2, op0, op1, R, W):
        def f(e):
            if s2 is None:
                return e.tensor_scalar(out=out, in0=in0, scalar1=s1, scalar2=None, op0=op0)
            return e.tensor_scalar(out=out, in0=in0, scalar1=s1, scalar2=s2, op0=op0, op1=op1)
        return self.op(eng, f, R, W)

    def tt(self, eng, out, in0, in1, op, R, W):
        return self.op(eng, lambda e: e.tensor_tensor(out=out, in0=in0, in1=in1, op=op), R, W)

    def stt(self, out, in0, scalar, in1, op0, op1, R, W):
        return self.op("dve", lambda e: e.scalar_tensor_tensor(out=out, in0=in0, scalar=scalar, in1=in1,
                                                              op0=op0, op1=op1), R, W)

    def recip(self, out, in_, R, W):
        return self.op("dve", lambda e: e.reciprocal(out=out, in_=in_), R, W)

    def copy(self, eng, out, in_, R, W):
        return self.op(eng, lambda e: e.tensor_copy(out=out, in_=in_), R, W)

    def memset(self, eng, ap, val, W):
        return self.op(eng, lambda e: e.memset(ap, val), (), W)

    def scan(self, out, d0, d1, init, R, W):
        return self.op("dve", lambda e: e.tensor_tensor_scan(out=out, data0=d0, data1=d1, initial=init,
                                                            op0=ALU.mult, op1=ALU.add), R, W)

    def ld(self, q, out, in_, R, W):
        return self.dma(q, lambda e: e.dma_start(out=out, in_=in_), R, W)


class Arena:
    def __init__(self, P, words):
        self.t = P.sb("arena", [128, words], F32)
        self.words = words
        self.off = 0

    def reset(self):
        self.off = 0

    def _take(self, words):
        o = self.off
        self.off += words
        assert self.off <= self.words, ("arena overflow", self.off, self.words)
        return o

    def f32(self, *shape):
        n = int(np.prod(shape))
        o = self._take(n)
        ap = self.t[:, o:o + n]
        return self._shape(ap, shape)

    def bf16(self, *shape):
        n = int(np.prod(shape))
        w = (n + 1) // 2
        o = self._take(w)
        ap = self.t[:, o:o + w].bitcast(BF16)[:, 0:n]
        return self._shape(ap, shape)

    @staticmethod
    def _shape(ap, shape):
        if len(shape) == 1:
            return ap
        if len(shape) == 2:
            return ap.rearrange("p (a b) -> p a b", a=shape[0], b=shape[1])
        if len(shape) == 3:
            return ap.rearrange("p (a b c) -> p a b c", a=shape[0], b=shape[1], c=shape[2])
        raise ValueError(shape)


def who_of(tt):
    return 2 if (tt % NT) < 2 else tt // NT


def build_program(n_layers=L_, stages=99, dump=(), LW=L_):
    nc = bass.Bass("TRN2", target_bir_lowering=False)
    L_ = LW
    dt_in = lambda name, shape: nc.dram_tensor(name, list(shape), F32, kind="ExternalInput").ap()

    def scr(name, shape, dtype=F32):
        kind = "ExternalOutput" if name in dump else "Internal"
        return nc.dram_tensor(name, list(shape), dtype, kind=kind).ap()

    xin = dt_in("xin", [NB, T, D])
    cvecT = dt_in("cvecT", [128, 16, 3])
    w_mod = dt_in("w_mod", [L_, D, 6 * D])
    b_mod = dt_in("b_mod", [L_, 6 * D])
    gT_in = dt_in("gT", [L_, 2, 128, 16])
    w_in = dt_in("w_in", [L_, D, INC])
    pv_in = dt_in("pv", [L_, 128, NPV])
    dflam = dt_in("df_lam", [L_, 256])
    nab = dt_in("nab", [L_, 4, 128, 5 * 5 * 128])
    rgw = dt_in("rgw", [L_, 8, 128, 4 * 128])
    w_br = dt_in("w_branch", [L_, D, D])
    w_out = dt_in("w_out", [L_, D, D])
    wr_in = dt_in("wr", [L_, 128, 16 * 36])
    br_in = dt_in("br", [L_, 36])
    w1 = dt_in("w1", [L_, 32, D, 512])
    w3 = dt_in("w3", [L_, 32, D, 512])
    w2 = dt_in("w2", [L_, 32, 512, D])
    cst = dt_in("cst", [128, 384])
    lcst_in = dt_in("lcst", [L_, 128, 2])
    rope = dt_in("rope", [2, 128, T])
    xout = nc.dram_tensor("xout", [NB, T, D], F32, kind="ExternalOutput").ap()

    naK = scr("naK", [NB, 512, T], BF16)
    naQ = scr("naQ", [NB, 512, T], BF16)
    naV = scr("naV", [NB, T, 512], BF16)
    dfK = scr("dfK", [NB, 512, T], BF16)
    dfQ = scr("dfQ", [NB, 512, T], BF16)
    dfV = scr("dfV", [NB, T, 512], BF16)
    rgx = scr("rgx", [NB, 1024, T], F32)
    rgg = scr("rgg", [NB, 1024, T], F32)
    gts = scr("gts", [NB, 3 * D, T], BF16)
    yT = scr("yT", [NB, D, T], BF16)
    h2T = scr("h2T", [NB, D, T], BF16)
    wcm = scr("wcm", [NB * NT, 128, 32], F32)
    gtrow = scr("gtrow", [2, 3, D], F32)

    P = Prog(nc)
    AR = Arena(P, 47616)
    psb = [P.ps("psb%d" % i, [128, 512], F32) for i in range(8)]
    PB = [Buf(excl=True) for _ in range(8)]
    c_f = P.sb("c_f", [128, 384], F32)
    c_b = P.sb("c_b", [128, 384], BF16)
    ones_b = P.sb("ones_b", [128, 128], BF16)
    scT = P.sb("scT", [128, 16, 3], BF16)
    modT = P.sb("modT", [128, 96, 3], F32)
    S12 = P.sb("S12", [128, 2, 16, 3], F32)
    pvt = P.sb("pvt", [128, NPV], F32)
    lam_t = P.sb("lam_t", [128, 8], F32)
    gsub = P.sb("gsub", [128, 1], F32)
    rgc = P.sb("rgc", [128, 2, 16], F32)
    CB = Buf()
    cbt = P.sb("cbt", [128, 2], F32)
    G["eps"] = cbt[:, 0:1]
    G["one"] = cbt[:, 1:2]
    ident = c_f[:, 0:128]
    rot_b = c_b[:, 128:256]
    blk_b = c_b[:, 256:384]

    P.ld("sp", c_f[:], cst, [], [CB])
    P.copy("dve", c_b[:], c_f[:], [CB], [CB])
    P.memset("dve", ones_b[:], 1.0, [CB])
    P.memset("dve", cbt[:, 0:1], EPS, [CB])
    P.memset("dve", cbt[:, 1:2], 1.0, [CB])
    AR.reset()
    cv = AR.f32(16, 3)
    cs = AR.f32(16, 3)
    TB = Buf()
    P.ld("sp", cv, cvecT, [], [TB])
    P.act(cs, cv, AF.Sigmoid, [TB], [TB])
    P.tt("dve", scT[:], cv, cs, ALU.mult, [TB], [CB])
    P.fence()

    for l in range(n_layers):
        lam_init = 0.8 - 0.6 * float(np.exp(-0.3 * l))
        xsrc = xin if l == 0 else xout

        AR.reset()
        MB = Buf()
        bmod_b = AR.bf16(6 * D)
        wsl = [AR.bf16(16, 512) for _ in range(2)]
        WS = bufs(2)
        g12 = AR.f32(2, 16)
        gsb = [AR.f32(512) for _ in range(2)]
        GS = bufs(2)
        dl = AR.f32(256)
        dtmp = AR.f32(64)
        P.ld("pool", bmod_b[0:1, :].rearrange("o (a b) -> o a b", b=512), b_mod[l:l + 1, :].rearrange("o (a b) -> o a b", b=512), [], [MB])
        P.ld("sp", g12, gT_in[l].rearrange("j p c -> p j c"), [], [MB])
        P.ld("sp", pvt[:], pv_in[l], [], [MB])
        P.ld("sp", dl, dflam[l:l + 1, :].broadcast_to([128, 256]), [], [MB])
        lct = AR.f32(2)
        P.ld("sp", lct, lcst_in[l], [], [MB])
        psM = psb[0]
        psG = [psb[1], psb[2]]
        ng = 0
        for s in range(24):
            wb = wsl[s % 2]
            P.ld("pool", wb, w_mod[l][:, s * 512:(s + 1) * 512].rearrange("(c p) n -> p c n", p=128), [], [WS[s % 2]])
            for cc in range(4):
                ch = s * 4 + cc
                for kc in range(16):
                    P.mm(psM[:, ch * 3:ch * 3 + 3], wb[:, kc, cc * 128:(cc + 1) * 128], scT[:, kc, :],
                         kc == 0, False, [WS[s % 2], CB], [PB[0]])
                P.mm(psM[:, ch * 3:ch * 3 + 3], bmod_b[0:1, ch * 128:(ch + 1) * 128], ones_b[0:1, 0:3],
                     False, True, [MB, CB], [PB[0]])
            if s // 4 in (2, 5):
                j = 0 if s // 4 == 2 else 1
                pg = psG[ng % 2]
                for kc in range(16):
                    P.mm(pg[0:3, :], scT[:, kc, :], wb[:, kc, :], kc == 0, False, [WS[s % 2], CB], [PB[1 + ng % 2]])
                P.mm(pg[0:3, :], ones_b[0:1, 0:3], bmod_b[0:1, s * 512:(s + 1) * 512], False, True,
                     [MB, CB], [PB[1 + ng % 2]])
                P.act(gsb[ng % 2][0:3, :], pg[0:3, :], AF.Copy, [PB[1 + ng % 2]], [GS[ng % 2]])
                P.ld("sp", gtrow[j, :, (s % 4) * 512:(s % 4 + 1) * 512], gsb[ng % 2][0:3, :], [GS[ng % 2]], [])
                ng += 1
        P.act(modT[:].rearrange("p a b -> p (a b)"), psM[:, 0:288], AF.Copy, [PB[0]], [MB])
        for j in range(2):
            for w in range(3):
                P.stt(S12[:, j, :, w], modT[:, (1 + 3 * j) * 16:(2 + 3 * j) * 16, w], 1.0, g12[:, j, :],
                      ALU.add, ALU.mult, [MB], [MB])
        P.tt("dve", dtmp, dl[:, 0:64], dl[:, 64:128], ALU.mult, [MB], [MB])
        P.op("dve", lambda e, o=lam_t[:, 2:3], i=dtmp: e.reduce_sum(out=o, in_=i, axis=mybir.AxisListType.X), [MB], [MB])
        P.tt("dve", dtmp, dl[:, 128:192], dl[:, 192:256], ALU.mult, [MB], [MB])
        P.op("dve", lambda e, o=lam_t[:, 3:4], i=dtmp: e.reduce_sum(out=o, in_=i, axis=mybir.AxisListType.X), [MB], [MB])
        P.act(lam_t[:, 4:6], lam_t[:, 2:4], AF.Exp, [MB], [MB])
        P.tt("dve", lam_t[:, 6:7], lam_t[:, 5:6], lam_t[:, 4:5], ALU.subtract, [MB], [MB])
        P.ts("dve", lam_t[:, 0:1], lam_t[:, 6:7], lct[:, 0:1], None, ALU.add, None, [MB], [MB])
        P.ts("dve", gsub[:], pvt[:, 4:5], lct[:, 1:2], None, ALU.mult, None, [MB], [MB])
        P.act(rgc[:, 0, :], pvt[:, 80:96], AF.Exp, [MB], [MB], scale=-1.0)
        P.act(rgc[:, 0, :], rgc[:, 0, :], AF.Ln, [MB], [MB], bias=G['one'])
        P.ts("dve", rgc[:, 1, :], rgc[:, 0, :], -16.0, None, ALU.mult, None, [MB], [MB])
        P.ts("dve", rgc[:, 0, :], rgc[:, 0, :], -8.0, None, ALU.mult, None, [MB], [MB])
        P.fence()
        if stages < 1:
            break

        AR.reset()
        cos_t = AR.f32(T)
        sin_t = AR.f32(T)
        RB = Buf()
        P.ld("sp", cos_t, rope[0], [], [RB])
        P.ld("sp", sin_t, rope[1], [], [RB])
        hT = AR.bf16(16, 768)
        HT = bufs(6)
        xt = [AR.f32(D) for _ in range(2)]
        XT = bufs(2)
        st = [AR.f32(4) for _ in range(2)]
        ST = bufs(2)
        wsl = [AR.bf16(16, 512) for _ in range(2)]
        WS = bufs(2)
        ev = [dict(sq=AR.bf16(384), zc=AR.f32(384), zg=AR.bf16(384), t1=AR.f32(384), t2=AR.f32(384),
                   rs=AR.f32(384), o16=AR.bf16(384), o32=AR.f32(384)) for _ in range(2)]
        EV = [dict(sq=Buf(), zc=Buf(), zg=Buf(), t1=Buf(), t2=Buf(), rs=Buf(), o16=Buf(), o32=Buf()) for _ in range(2)]
        vo = [AR.bf16(512) for _ in range(2)]
        VO = bufs(2)
        junk = AR.f32(D); JK = Buf()
        nld = 0
        nev = 0
        nvo = 0
        nx = 0
        for blk in range(DEBUG.get('pblks', 6)):
            b = blk // 3
            t0 = (blk % 3) * 768
            for i in range(6):
                tt_ = blk * 6 + i
                w = who_of(tt_)
                x_ = xt[nx % 2]; X_ = XT[nx % 2]; s_ = st[nx % 2]; S_ = ST[nx % 2]
                nx += 1
                P.ld("sp", x_, xsrc[b, t0 + i * 128:t0 + (i + 1) * 128, :], [], [X_])
                norm_mod_T(P, x_, X_, s_, S_, psb, PB, ident, CB, S12[:, 0, :, w], modT[:, 0:16, w], MB,
                           hT[:, :, i * 128:(i + 1) * 128], HT[i], junk, JK)
            for s in DEBUG.get('pslabs', range(22)):
                wb = wsl[nld % 2]; WB = WS[nld % 2]
                nld += 1
                P.ld("pool", wb, w_in[l][:, s * 512:(s + 1) * 512].rearrange("(c p) n -> p c n", p=128), [], [WB])
                if s in (1, 3):
                    dst = naV if s == 1 else dfV
                    for i in range(6):
                        pb_i = 6 + (nvo % 2)
                        for kc in range(16):
                            P.mm(psb[pb_i][:, :], hT[:, kc, i * 128:(i + 1) * 128], wb[:, kc, :], kc == 0, kc == 15,
                                 [HT[i], WB], [PB[pb_i]])
                        P.act(vo[nvo % 2], psb[pb_i][:, :], AF.Copy, [PB[pb_i]], [VO[nvo % 2]])
                        P.ld("sp", dst[b, t0 + i * 128:t0 + (i + 1) * 128, :], vo[nvo % 2], [VO[nvo % 2]], [])
                        nvo += 1
                    continue
                for cc in range(4):
                    for hf in range(2):
                        pi = nev % 2
                        E = ev[pi]; EB = EV[pi]
                        nev += 1
                        pz = psb[pi]
                        tk = slice(t0 + hf * 384, t0 + (hf + 1) * 384)
                        for kc in range(16):
                            P.mm(pz[:, 0:384], wb[:, kc, cc * 128:(cc + 1) * 128], hT[:, kc, hf * 384:(hf + 1) * 384],
                                 kc == 0, kc == 15, HT[hf * 3:hf * 3 + 3] + [WB], [PB[pi]])
                        if s in (0, 6):
                            dst = naK if s == 0 else naQ
                            gcol = pvt[:, 1:2] if s == 0 else pvt[:, 0:1]
                            P.act(E["sq"], pz[:, 0:384], AF.Square, [PB[pi]], [EB["sq"]])
                            P.ts("dve", E["zc"], pz[:, 0:384], gcol, None, ALU.mult, None, [PB[pi], MB], [EB["zc"]])
                            pn = psb[2 + pi]
                            P.mm(pn[:, 0:384], ones_b[:], E["sq"], True, True, [EB["sq"], CB], [PB[2 + pi]])
                            P.act(E["rs"], pn[:, 0:384], AF.Sqrt, [PB[2 + pi]], [EB["rs"]], scale=1.0 / 128, bias=G['eps'])
                            P.recip(E["rs"], E["rs"], [EB["rs"]], [EB["rs"]])
                            P.tt(PEW, E["o16"], E["zc"], E["rs"], ALU.mult, [EB["zc"], EB["rs"]], [EB["o16"]])
                            P.ld("sp", dst[b, cc * 128:(cc + 1) * 128, tk], E["o16"], [EB["o16"]], [])
                        elif s in (2, 7):
                            dst = dfK if s == 2 else dfQ
                            gcol = pvt[:, 3:4] if s == 2 else pvt[:, 2:3]
                            P.act(E["sq"], pz[:, 0:384], AF.Square, [PB[pi]], [EB["sq"]])
                            P.ts("dve", E["zg"], pz[:, 0:384], gcol, None, ALU.mult, None, [PB[pi], MB], [EB["zg"]])
                            pn = psb[2 + pi]
                            pr = psb[4 + pi]
                            P.mm(pn[:, 0:384], blk_b, E["sq"], True, True, [EB["sq"], CB], [PB[2 + pi]])
                            dfm = DEBUG.get("dfmode", 0)
                            if dfm < 2:
                                P.mm(pr[:, 0:384], rot_b, E["zg"], True, True, [EB["zg"], CB], [PB[4 + pi]])
                            P.tt(PEW, E["t1"], E["zg"], cos_t[:, tk], ALU.mult, [EB["zg"], RB], [EB["t1"]])
                            if dfm < 1:
                                P.tt("dve", E["t2"], pr[:, 0:384], sin_t[:, tk], ALU.mult, [PB[4 + pi], RB], [EB["t2"]])
                                P.tt(PEW, E["t1"], E["t1"], E["t2"], ALU.add, [EB["t1"], EB["t2"]], [EB["t1"]])
                            P.act(E["rs"], pn[:, 0:384], AF.Sqrt, [PB[2 + pi]], [EB["rs"]], scale=1.0 / 64, bias=G['eps'])
                            P.recip(E["rs"], E["rs"], [EB["rs"]], [EB["rs"]])
                            P.tt("dve", E["o16"], E["t1"], E["rs"], ALU.mult, [EB["t1"], EB["rs"]], [EB["o16"]])
                            P.ld("sp", dst[b, cc * 128:(cc + 1) * 128, tk], E["o16"], [EB["o16"]], [])
                            if DEBUG.get("ufence"):
                                P.fence()
                        elif s in (4, 5, 8, 9):
                            dst = rgx if s in (4, 5) else rgg
                            r0 = ((s - 4) % 4) * 512 + cc * 128 if s in (4, 5) else (s - 8) * 512 + cc * 128
                            P.act(E["o32"], pz[:, 0:384], AF.Copy, [PB[pi]], [EB["o32"]])
                            P.ld("sp", dst[b, r0:r0 + 128, tk], E["o32"], [EB["o32"]], [])
                        else:
                            r0 = (s - 10) * 512 + cc * 128
                            P.act(E["o16"], pz[:, 0:384], AF.Sigmoid, [PB[pi]], [EB["o16"]])
                            P.ld("sp", gts[b, r0:r0 + 128, tk], E["o16"], [EB["o16"]], [])
        P.fence()
        if stages < 2:
            break

        na_scale = 128.0 ** -0.5
        for b in range(NB):
            for h in range(4):
                AR.reset()
                kT = AR.bf16(T); qT = AR.bf16(T); V = AR.bf16(NT, 128); bia = AR.f32(5, 5, 128)
                ya = AR.bf16(T)
                LB = Buf(); YA = Buf()
                P.ld("sp", kT, naK[b, h * 128:(h + 1) * 128, :], [], [LB])
                P.ld("sp", qT, naQ[b, h * 128:(h + 1) * 128, :], [], [LB])
                P.ld("sp", V, naV[b, :, h * 128:(h + 1) * 128].rearrange("(t p) e -> p t e", p=128), [], [LB])
                P.ld("sp", bia.rearrange("p a b c -> p (a b c)"), nab[l, h], [], [LB])
                tmp = [AR.f32(640) for _ in range(2)]; TM = bufs(2)
                PT = [AR.bf16(896) for _ in range(2)]; PTB = bufs(2)
                rsm = [AR.f32(128) for _ in range(2)]; RS = bufs(2)
                for qi in range(NT):
                    pi = qi % 2
                    qc = slice(qi * 128, (qi + 1) * 128)
                    pA, pB_, pO, pS = psb[pi * 4], psb[pi * 4 + 1], psb[pi * 4 + 2], psb[pi * 4 + 3]
                    BA, BB, BO, BS = PB[pi * 4], PB[pi * 4 + 1], PB[pi * 4 + 2], PB[pi * 4 + 3]
                    if qi < 2:
                        for c in range(2):
                            P.mm(pA[:, c * 128:(c + 1) * 128], kT[:, c * 128:(c + 1) * 128], qT[:, qc], True, True, [LB], [BA])
                        P.act(PT[pi][:, 0:256], pA[:, 0:256], AF.Exp, [BA], [PTB[pi]], scale=na_scale)
                        vts = [0, 1]
                    else:
                        j = qi - 2
                        ks = min(max(2 * j - 4, 0), 22)
                        ty = 0 if j == 0 else 1 if j == 1 else 3 if j == 14 else 4 if j == 15 else 2
                        k0 = 256 + ks * 64
                        for c in range(5):
                            tgt = pA[:, c * 128:(c + 1) * 128] if c < 4 else pB_[:, 0:128]
                            P.mm(tgt, kT[:, k0 + c * 128:k0 + (c + 1) * 128], qT[:, qc], True, True, [LB], [BA if c < 4 else BB])
                        for c in range(2):
                            P.mm(pB_[:, (1 + c) * 128:(2 + c) * 128], kT[:, c * 128:(c + 1) * 128], qT[:, qc], True, True, [LB], [BB])
                        P.stt(tmp[pi][:, 0:512], pA[:, 0:512], na_scale, bia[:, ty, 0:4, :].rearrange("p a b -> p (a b)"),
                              ALU.mult, ALU.add, [BA, LB], [TM[pi]])
                        P.stt(tmp[pi][:, 512:640], pB_[:, 0:128], na_scale, bia[:, ty, 4, :], ALU.mult, ALU.add, [BB, LB], [TM[pi]])
                        P.act(PT[pi][:, 0:640], tmp[pi][:, 0:640], AF.Exp, [TM[pi]], [PTB[pi]])
                        P.act(PT[pi][:, 640:896], pB_[:, 128:384], AF.Exp, [BB], [PTB[pi]], scale=na_scale)
                        vts = [2 + ks // 2 + c for c in range(5)] + [0, 1]
                    n = len(vts)
                    for c, vt in enumerate(vts):
                        P.mm(pO[:, 0:128], V[:, vt, :], PT[pi][:, c * 128:(c + 1) * 128], c == 0, c == n - 1, [LB, PTB[pi]], [BO])
                    for c, vt in enumerate(vts):
                        P.mm(pS[:, 0:128], ones_b[:], PT[pi][:, c * 128:(c + 1) * 128], c == 0, c == n - 1, [CB, PTB[pi]], [BS])
                    P.recip(rsm[pi], pS[:, 0:128], [BS], [RS[pi]])
                    P.tt("dve", ya[:, qc], pO[:, 0:128], rsm[pi], ALU.mult, [BO, RS[pi]], [YA])
                P.ld("sp", yT[b, h * 128:(h + 1) * 128, :], ya, [YA], [])
                P.fence()
        if stages < 3:
            break

        for b in range(NB):
            for h in range(4):
                AR.reset()
                kT = [AR.bf16(T) for _ in range(2)]; qT = [AR.bf16(T) for _ in range(2)]
                V = AR.bf16(NT, 128); yb = AR.bf16(T)
                LB = Buf(); YB = Buf()
                for m in range(2):
                    r0 = h * 128 + m * 64
                    P.ld("sp", kT[m][0:64, :], dfK[b, r0:r0 + 64, :], [], [LB])
                    P.ld("sp", qT[m][0:64, :], dfQ[b, r0:r0 + 64, :], [], [LB])
                P.ld("sp", V, dfV[b, :, h * 128:(h + 1) * 128].rearrange("(t p) e -> p t e", p=128), [], [LB])
                PT = [AR.bf16(512) for _ in range(3)]; PTB = bufs(3)
                r_ = [AR.f32(512) for _ in range(2)]; RB_ = bufs(2)
                t_ = [AR.f32(512) for _ in range(2)]; TB_ = bufs(2)
                o_ = AR.f32(512); OB = Buf()
                sq = AR.bf16(512); SQ = Buf()
                rs = AR.f32(512); RSB = Buf()
                npt = 0
                for qb in range(5):
                    if qb == 0:
                        q0, N, kcs = 0, 256, [0, 1]
                    else:
                        q0, N, kcs = 256 + (qb - 1) * 512, 512, list(range(NT))
                    for m in range(2):
                        pO, pS = psb[3 + 2 * m], psb[4 + 2 * m]
                        BO, BS = PB[3 + 2 * m], PB[4 + 2 * m]
                        for ci, kc in enumerate(kcs):
                            pi = npt % 3
                            npt += 1
                            P.mm(psb[pi][:, 0:N], kT[m][0:64, kc * 128:(kc + 1) * 128], qT[m][0:64, q0:q0 + N], True, True, [LB], [PB[pi]])
                            P.act(PT[pi][:, 0:N], psb[pi][:, 0:N], AF.Exp, [PB[pi]], [PTB[pi]], scale=0.125)
                            P.mm(pO[:, 0:N], V[:, kc, :], PT[pi][:, 0:N], ci == 0, ci == len(kcs) - 1, [LB, PTB[pi]], [BO])
                            P.mm(pS[:, 0:N], ones_b[:], PT[pi][:, 0:N], ci == 0, ci == len(kcs) - 1, [CB, PTB[pi]], [BS])
                        P.recip(r_[m][:, 0:N], pS[:, 0:N], [BS], [RB_[m]])
                        P.tt("dve", t_[m][:, 0:N], pO[:, 0:N], r_[m][:, 0:N], ALU.mult, [BO, RB_[m]], [TB_[m]])
                    P.stt(o_[:, 0:N], t_[1][:, 0:N], lam_t[:, 0:1], t_[0][:, 0:N], ALU.mult, ALU.add, [TB_[0], TB_[1], MB], [OB])
                    P.act(sq[:, 0:N], o_[:, 0:N], AF.Square, [OB], [SQ])
                    P.mm(psb[7][:, 0:N], ones_b[:], sq[:, 0:N], True, True, [SQ, CB], [PB[7]])
                    P.act(rs[:, 0:N], psb[7][:, 0:N], AF.Sqrt, [PB[7]], [RSB], scale=1.0 / 128, bias=G['eps'])
                    P.recip(rs[:, 0:N], rs[:, 0:N], [RSB], [RSB])
                    P.stt(yb[:, q0:q0 + N], o_[:, 0:N], gsub[:, 0:1], rs[:, 0:N], ALU.mult, ALU.mult, [OB, RSB, MB], [YB])
                P.ld("sp", yT[b, 512 + h * 128:512 + (h + 1) * 128, :], yb, [YB], [])
                P.fence()
        if stages < 4:
            break

        for b in range(NB):
            for n in range(8):
                AR.reset()
                zc = AR.f32(CTX + 3); zl = AR.f32(SEQ + 3)
                ZB = Buf()
                wrg = AR.bf16(4, 128); WB = Buf()
                u = AR.f32(T); UB = Buf()
                ub = AR.bf16(T); UBB = Buf()
                gate = AR.f32(T); GB = Buf()
                r_ = AR.f32(T); RB_ = Buf()
                i_ = AR.f32(T); IB = Buf()
                a_ = AR.f32(T); AB = Buf()
                m_ = AR.f32(T); MB2 = Buf()
                hs = [AR.f32(T) for _ in range(2)]; HB = bufs(2)
                yc = AR.bf16(T); YC = Buf()
                P.memset(PEW, zc, 0.0, [ZB])
                P.memset(PEW, zl, 0.0, [ZB])
                P.ld("sp", zc[:, 2:2 + CTX], rgx[b, n * 128:(n + 1) * 128, 0:CTX], [], [ZB])
                P.ld("sp", zl[:, 2:2 + SEQ], rgx[b, n * 128:(n + 1) * 128, CTX:T], [], [ZB])
                P.ld("sp", gate, rgg[b, n * 128:(n + 1) * 128, :], [], [GB])
                P.ld("pool", wrg.rearrange("p a b -> p (a b)"), rgw[l, n], [], [WB])
                for (z, o0, N) in ((zc, 0, CTX), (zl, CTX, SEQ)):
                    P.ts("dve", u[:, o0:o0 + N], z[:, 0:N], pvt[:, 8 + n:9 + n], pvt[:, 40 + n:41 + n], ALU.mult, ALU.add, [ZB, MB], [UB])
                    for jj in range(1, 4):
                        P.stt(u[:, o0:o0 + N], z[:, jj:jj + N], pvt[:, 8 + jj * 8 + n:9 + jj * 8 + n], u[:, o0:o0 + N],
                              ALU.mult, ALU.add, [ZB, MB], [UB])
                P.copy(PEW, ub, u, [UB], [UBB])
                for d in range(2):
                    for (wi, dstt, DB, bcol) in ((0, r_, RB_, 48 + d * 8 + n), (1, i_, IB, 64 + d * 8 + n)):
                        for cblk in range(5):
                            c0 = cblk * 512
                            N = min(512, T - c0)
                            pi = (cblk + wi) % 2
                            P.mm(psb[pi][:, 0:N], wrg[:, d * 2 + wi, :], ub[:, c0:c0 + N], True, True, [WB, UBB], [PB[pi]])
                            P.act(dstt[:, c0:c0 + N], psb[pi][:, 0:N], AF.Sigmoid, [PB[pi], MB], [DB], bias=pvt[:, bcol:bcol + 1])
                    P.act(a_, r_, AF.Exp, [RB_, MB], [AB], scale=rgc[:, 0, d * 8 + n:d * 8 + n + 1])
                    P.act(m_, r_, AF.Exp, [RB_, MB], [MB2], scale=rgc[:, 1, d * 8 + n:d * 8 + n + 1])
                    P.ts("dve", m_, m_, -1.0, 1.0, ALU.mult, ALU.add, [MB2], [MB2])
                    P.act(m_, m_, AF.Sqrt, [MB2], [MB2])
                    P.tt(PEW, i_, i_, u, ALU.mult, [IB, UB], [IB])
                    P.tt("dve", m_, m_, i_, ALU.mult, [MB2, IB], [MB2])
                    if d == 0:
                        P.scan(hs[0], a_, m_, 0.0, [AB, MB2], [HB[0]])
                    else:
                        P.scan(hs[1][:, 0:CTX][:, ::-1], a_[:, 0:CTX][:, ::-1], m_[:, 0:CTX][:, ::-1], 0.0, [AB, MB2], [HB[1]])
                        P.scan(hs[1][:, CTX:T][:, ::-1], a_[:, CTX:T][:, ::-1], m_[:, CTX:T][:, ::-1], hs[1][:, 0:1],
                               [AB, MB2, HB[1]], [HB[1]])
                P.tt(PEW, hs[0], hs[0], hs[1], ALU.add, [HB[0], HB[1]], [HB[0]])
                P.act(r_, gate, AF.Square, [GB], [RB_])
                P.ts("dve", r_, r_, 0.044715, 1.0, ALU.mult, ALU.add, [RB_], [RB_])
                P.tt("dve", r_, r_, gate, ALU.mult, [RB_, GB], [RB_])
                P.act(r_, r_, AF.Sigmoid, [RB_], [RB_], scale=1.5957691216057308)
                P.tt(PEW, r_, r_, gate, ALU.mult, [RB_, GB], [RB_])
                P.tt("dve", yc, r_, hs[0], ALU.mult, [RB_, HB[0]], [YC])
                P.ld("sp", yT[b, 1024 + n * 128:1024 + (n + 1) * 128, :], yc, [YC], [])
                P.fence()
        if stages < 5:
            break

        AR.reset()
        gtb = AR.f32(3, D); GTB = Buf()
        P.ld("sp", gtb.rearrange("p a b -> p (a b)"), gtrow[0:1].rearrange("o w d -> o (w d)").broadcast_to([128, 3 * D]), [], [GTB])
        wr_t = AR.f32(16, 36); WRB = Buf()
        br_t = AR.f32(36)
        P.ld("sp", wr_t.rearrange("p a b -> p (a b)"), wr_in[l], [], [WRB])
        P.ld("sp", br_t, br_in[l:l + 1, :].broadcast_to([128, 36]), [], [WRB])
        ysb = AR.bf16(16, 768); YS = Buf()
        mT = AR.bf16(16, 768); MT = bufs(16)
        wsl = [AR.bf16(16, 512) for _ in range(2)]; WS = bufs(2)
        gsb3 = [AR.bf16(3, 768) for _ in range(2)]; G3 = bufs(2)
        ta = [AR.f32(384) for _ in range(2)]; TA = bufs(2)
        tb = [AR.f32(384) for _ in range(2)]; TBb = bufs(2)
        xt = [AR.f32(D) for _ in range(2)]; XT = bufs(2)
        junk = AR.f32(D); JK = Buf()
        st = [AR.f32(4) for _ in range(2)]; ST = bufs(2)
        h32 = [AR.f32(16, 128) for _ in range(2)]; H32 = bufs(2)
        h16 = [AR.bf16(16, 128) for _ in range(2)]; H16 = bufs(2)
        rt = [AR.f32(80) for _ in range(2)]; RT = bufs(2)
        nld = 0; ngl = 0; nu = 0; nx = 0
        for blk in range(6):
            b = blk // 3
            t0 = (blk % 3) * 768
            P.ld("sp", ysb, yT[b, :, t0:t0 + 768].rearrange("(c p) t -> p c t", p=128), [], [YS])
            for g in range(4):
                wb = wsl[nld % 2]; WB = WS[nld % 2]; nld += 1
                P.ld("pool", wb, w_br[l][:, g * 512:(g + 1) * 512].rearrange("(c p) n -> p c n", p=128), [], [WB])
                for cc in range(4):
                    dc = g * 4 + cc
                    g3 = gsb3[ngl % 2]; G3B = G3[ngl % 2]; ngl += 1
                    P.ld("sp", g3, gts[b, :, t0:t0 + 768].rearrange("(j r) t -> r j t", j=3)[dc * 128:(dc + 1) * 128], [], [G3B])
                    for hf in range(2):
                        pi = nu % 2; nu += 1
                        tk = slice(hf * 384, (hf + 1) * 384)
                        pa, pb2, pc = psb[pi * 3], psb[pi * 3 + 1], psb[pi * 3 + 2]
                        for (pp, BBp, k0, k1) in ((pa, PB[pi * 3], 0, 4), (pb2, PB[pi * 3 + 1], 4, 8), (pc, PB[pi * 3 + 2], 8, 16)):
                            for kc in range(k0, k1):
                                P.mm(pp[:, 0:384], wb[:, kc, cc * 128:(cc + 1) * 128], ysb[:, kc, tk], kc == k0, kc == k1 - 1, [WB, YS], [BBp])
                        P.tt("dve", ta[pi], pa[:, 0:384], g3[:, 0, tk], ALU.mult, [PB[pi * 3], G3B], [TA[pi]])
                        P.tt("dve", tb[pi], pb2[:, 0:384], g3[:, 1, tk], ALU.mult, [PB[pi * 3 + 1], G3B], [TBb[pi]])
                        P.tt(PEW, ta[pi], ta[pi], tb[pi], ALU.add, [TA[pi], TBb[pi]], [TA[pi]])
                        P.tt("dve", tb[pi], pc[:, 0:384], g3[:, 2, tk], ALU.mult, [PB[pi * 3 + 2], G3B, TA[pi]], [TBb[pi]])
                        P.tt(PEW, mT[:, dc, tk], ta[pi], tb[pi], ALU.add, [TA[pi], TBb[pi]], [MT[dc]])
            XO = bufs(6)
            for ps_ in range(2):
                wos = []
                for g in (2 * ps_, 2 * ps_ + 1):
                    wb = wsl[nld % 2]; WB = WS[nld % 2]; nld += 1
                    wos.append((g, wb, WB))
                    P.ld("pool", wb, w_out[l][:, g * 512:(g + 1) * 512].rearrange("(c p) n -> p c n", p=128), [], [WB])
                for i in range(6):
                    tt_ = blk * 6 + i
                    w = who_of(tt_)
                    x_ = xt[nx % 2]; X_ = XT[nx % 2]; nx += 1
                    hs_ = slice(ps_ * 1024, (ps_ + 1) * 1024)
                    P.ld("sp", x_[:, 0:1024], xsrc[b, t0 + i * 128:t0 + (i + 1) * 128, hs_], [XO[i]], [X_])
                    for (gg, wbb, WBB) in wos:
                        pi = 6 + (gg % 2)
                        for kc in range(16):
                            P.mm(psb[pi][:, :], mT[:, kc, i * 128:(i + 1) * 128], wbb[:, kc, :], kc == 0, kc == 15, MT + [WBB], [PB[pi]])
                        lo = (gg % 2) * 512
                        P.tt("dve", x_[:, 1024 + lo:1024 + lo + 512], psb[pi][:, :], gtb[:, w, gg * 512:(gg + 1) * 512], ALU.mult, [PB[pi], GTB], [X_])
                        P.tt(PEW, x_[:, lo:lo + 512], x_[:, lo:lo + 512], x_[:, 1024 + lo:1024 + lo + 512], ALU.add, [X_], [X_])
                    P.ld("sp", xout[b, t0 + i * 128:t0 + (i + 1) * 128, hs_], x_[:, 0:1024], [X_], [XO[i]])
            for i in range(6):
                tt_ = blk * 6 + i
                w = who_of(tt_)
                ix = i % 2
                x_ = xt[nx % 2]; X_ = XT[nx % 2]; nx += 1
                P.ld("sp", x_, xout[b, t0 + i * 128:t0 + (i + 1) * 128, :], [XO[i]], [X_])
                norm_mod_T(P, x_, X_, st[ix], ST[ix], psb, PB, ident, CB, S12[:, 1, :, w], modT[:, 48:64, w], MB,
                           h32[ix], H32[ix], junk, JK, banks=(0, 1, 2, 3))
                P.copy(PEW, h16[ix], h32[ix], [H32[ix]], [H16[ix]])
                P.ld("sp", h2T[b, :, t0 + i * 128:t0 + (i + 1) * 128].rearrange("(c p) t -> p c t", p=128), h16[ix], [H16[ix]], [])
                pr = psb[4 + ix]
                for kc in range(16):
                    P.mm(pr[:, 0:36], h32[ix][:, kc, :], wr_t[:, kc, :], kc == 0, kc == 15, [H32[ix], WRB], [PB[4 + ix]])
                route(P, pr[:, 0:36], PB[4 + ix], br_t, WRB, rt[ix], RT[ix])
                P.ld("sp", wcm[tt_], rt[ix][:, 0:32], [RT[ix]], [])
        P.fence()
        if stages < 6:
            break

        AR.reset()
        gtb = AR.f32(3, D); GTB = Buf()
        P.ld("sp", gtb.rearrange("p a b -> p (a b)"), gtrow[1:2].rearrange("o w d -> o (w d)").broadcast_to([128, 3 * D]), [], [GTB])
        hb = AR.bf16(16, 768); HBB = Buf()
        acc = AR.f32(6, D); ACC = bufs(6)
        wc = AR.f32(6, 32); WCB = Buf()
        w13 = [AR.bf16(16, 256) for _ in range(4)]; W13 = bufs(4)
        w2s = [AR.bf16(4, D) for _ in range(2)]; W2 = bufs(2)
        hid = AR.bf16(4, 768); HID = bufs(4)
        sl = [AR.f32(384) for _ in range(2)]; SL = bufs(2)
        xt = [AR.f32(D) for _ in range(2)]; XT = bufs(2)
        n13 = 0; n2 = 0; nu = 0; nx = 0
        last = False
        for blk in range(6):
            b = blk // 3
            t0 = (blk % 3) * 768
            P.ld("sp", hb, h2T[b, :, t0:t0 + 768].rearrange("(c p) t -> p c t", p=128), [], [HBB])
            P.ld("sp", wc, wcm[blk * 6:(blk + 1) * 6].rearrange("t p e -> p t e"), [], [WCB])
            for e_ in range(32):
                w2b = w2s[n2 % 2]; W2B = W2[n2 % 2]; n2 += 1
                for hc in range(2):
                    wa = w13[n13 % 4]; WA = W13[n13 % 4]; n13 += 1
                    wb3 = w13[n13 % 4]; WB3 = W13[n13 % 4]; n13 += 1
                    P.ld("pool", wa, w1[l, e_][:, hc * 256:(hc + 1) * 256].rearrange("(c p) n -> p c n", p=128), [], [WA])
                    P.ld("pool", wb3, w3[l, e_][:, hc * 256:(hc + 1) * 256].rearrange("(c p) n -> p c n", p=128), [], [WB3])
                    if hc == 0:
                        P.ld("pool", w2b, w2[l, e_].rearrange("(c p) n -> p c n", p=128), [], [W2B])
                    for c2 in range(2):
                        c = hc * 2 + c2
                        for hf in range(2):
                            pi = nu % 2; nu += 1
                            tk = slice(hf * 384, (hf + 1) * 384)
                            p1, p3 = psb[pi * 2], psb[pi * 2 + 1]
                            for kc in range(16):
                                P.mm(p1[:, 0:384], wa[:, kc, c2 * 128:(c2 + 1) * 128], hb[:, kc, tk], kc == 0, kc == 15, [WA, HBB], [PB[pi * 2]])
                            for kc in range(16):
                                P.mm(p3[:, 0:384], wb3[:, kc, c2 * 128:(c2 + 1) * 128], hb[:, kc, tk], kc == 0, kc == 15, [WB3, HBB], [PB[pi * 2 + 1]])
                            P.act(sl[pi], p1[:, 0:384], AF.Silu, [PB[pi * 2]], [SL[pi]])
                            P.tt("dve", hid[:, c, tk], p3[:, 0:384], sl[pi], ALU.mult, [PB[pi * 2 + 1], SL[pi]], [HID[c]])
                for i in range(6):
                    for g in range(4):
                        pi = 4 + g
                        for c in range(4):
                            P.mm(psb[pi][:, :], hid[:, c, i * 128:(i + 1) * 128], w2b[:, c, g * 512:(g + 1) * 512], c == 0, c == 3, HID + [W2B], [PB[pi]])
                        cs_ = slice(g * 512, (g + 1) * 512)
                        if e_ == 0:
                            P.ts("dve", acc[:, i, cs_], psb[pi][:, :], wc[:, i, e_:e_ + 1], None, ALU.mult, None, [PB[pi], WCB], [ACC[i]])
                        else:
                            P.stt(acc[:, i, cs_], psb[pi][:, :], wc[:, i, e_:e_ + 1], acc[:, i, cs_], ALU.mult, ALU.add, [PB[pi], WCB], [ACC[i]])
            for i in range(6):
                tt_ = blk * 6 + i
                w = who_of(tt_)
                x_ = xt[nx % 2]; X_ = XT[nx % 2]; nx += 1
                XOB = Buf()
                P.ld("sp", x_, xout[b, t0 + i * 128:t0 + (i + 1) * 128, :], [XOB], [X_])
                P.tt(PEW, acc[:, i, :], acc[:, i, :], gtb[:, w, :], ALU.mult, [ACC[i], GTB], [ACC[i]])
                P.tt(PEW, x_, x_, acc[:, i, :], ALU.add, [X_, ACC[i]], [X_])
                P.ld("sp", xout[b, t0 + i * 128:t0 + (i + 1) * 128, :], x_, [X_], [XOB])
        P.fence()

    toks = P.all_tokens()
    waits = P._waits("sp", (), (), toks)
    P.ops["sp"].append((waits, None, None, 0))
    P.emit()
    return nc


def norm_mod_T(P, x_, X_, s_, S_, psb, PB, ident, CB, Scol, Bcol, MB, out, OB, scratch, SCR, banks=(4, 5, 6, 7)):
    P.tt("dve", scratch, x_, x_, ALU.mult, [X_], [SCR])
    P.op("dve", lambda e: e.reduce_sum(out=s_[:, 0:1], in_=scratch, axis=mybir.AxisListType.X), [SCR], [S_])
    P.act(s_[:, 1:2], s_[:, 0:1], AF.Sqrt, [S_], [S_], scale=1.0 / D, bias=G['eps'])
    P.recip(s_[:, 2:3], s_[:, 1:2], [S_], [S_])
    P.ts("dve", x_, x_, s_[:, 2:3], None, ALU.mult, None, [X_, S_], [X_])
    for q4 in range(4):
        bk = banks[q4]
        for k in range(4):
            kc = q4 * 4 + k
            P.tr(psb[bk][:, k * 128:(k + 1) * 128], x_[:, kc * 128:(kc + 1) * 128], ident, [X_, CB], [PB[bk]])
        for k in range(4):
            kc = q4 * 4 + k
            P.act(out[:, kc, :], psb[bk][:, k * 128:(k + 1) * 128], AF.Identity, [PB[bk], MB], [OB],
                  scale=Scol[:, kc:kc + 1], bias=Bcol[:, kc:kc + 1])


def route(P, logits_ps, LPB, br_t, WRB, rt, RT):
    lg = rt[:, 32:68]
    P.tt("dve", lg, logits_ps, br_t, ALU.add, [LPB, WRB], [RT])
    gm = rt[:, 68:69]; gs = rt[:, 69:70]; m1 = rt[:, 70:71]; m2 = rt[:, 71:72]
    sel = rt[:, 72:76]; ex = rt[:, 76:80]
    P.op("dve", lambda e: e.reduce_max(out=gm, in_=lg[:, 0:4], axis=mybir.AxisListType.X), [RT], [RT])
    P.ts("dve", sel, lg[:, 0:4], gm, None, ALU.is_ge, None, [RT], [RT])
    P.ts("dve", ex, lg[:, 0:4], gm, None, ALU.subtract, None, [RT], [RT])
    P.act(ex, ex, AF.Exp, [RT], [RT])
    P.op("dve", lambda e: e.reduce_sum(out=gs, in_=ex, axis=mybir.AxisListType.X), [RT], [RT])
    P.recip(gs, gs, [RT], [RT])
    P.ts("dve", sel, sel, 1.0, 1e30, ALU.subtract, ALU.mult, [RT], [RT])
    me = rt[:, 0:32]
    P.tt("dve", me.rearrange("p (g e) -> p g e", g=4), lg[:, 4:36].rearrange("p (g e) -> p g e", g=4),
         sel.unsqueeze(2).to_broadcast([128, 4, 8]), ALU.add, [RT], [RT])
    P.op("dve", lambda e: e.reduce_max(out=m1, in_=me, axis=mybir.AxisListType.X), [RT], [RT])
    s1 = lg[:, 4:36]
    P.ts("dve", s1, me, m1, None, ALU.is_ge, None, [RT], [RT])
    P.stt(me, s1, -1e30, me, ALU.mult, ALU.add, [RT], [RT])
    P.op("dve", lambda e: e.reduce_max(out=m2, in_=me, axis=mybir.AxisListType.X), [RT], [RT])
    s2 = rt[:, 0:32]
    P.tt("dve", ex[:, 0:1], m2, m1, ALU.subtract, [RT], [RT])
    P.act(ex[:, 0:1], ex[:, 0:1], AF.Exp, [RT], [RT])
    P.ts("dve", ex[:, 1:2], ex[:, 0:1], 1.0, None, ALU.add, None, [RT], [RT])
    P.recip(ex[:, 1:2], ex[:, 1:2], [RT], [RT])
    P.tt("dve", ex[:, 2:3], ex[:, 0:1], ex[:, 1:2], ALU.mult, [RT], [RT])
    P.tt("dve", ex[:, 1:2], ex[:, 1:2], gs, ALU.mult, [RT], [RT])
    P.tt("dve", ex[:, 2:3], ex[:, 2:3], gs, ALU.mult, [RT], [RT])
    P.ts("dve", s2, me, m2, ex[:, 2:3], ALU.is_ge, ALU.mult, [RT], [RT])
    P.stt(s2, s1, ex[:, 1:2], s2, ALU.mult, ALU.add, [RT], [RT])


def _host_consts():
    ident = np.eye(128, dtype=np.float32)
    rot = np.zeros((128, 128), np.float32)
    for m in range(128):
        k = m + 16 if (m % 32) < 16 else m - 16
        rot[k, m] = 1.0
    blk = np.zeros((128, 128), np.float32)
    blk[:64, :64] = 1.0
    blk[64:, 64:] = 1.0
    cst = np.concatenate([ident, rot, blk], axis=1)
    p = np.arange(128)
    d = p % 64
    half = d // 32
    f = d % 16
    sign = np.where((d % 32) < 16, -1.0, 1.0)
    inv = (10000.0 ** (-np.arange(16, dtype=np.float32) / 16)).astype(np.float32)
    s = np.arange(SEQ)
    pos = np.stack([s // 64, s % 64], 0).astype(np.float32)
    ang = pos[half][:, :] * inv[f][:, None]
    rope = np.zeros((2, 128, T), np.float32)
    rope[0, :, :CTX] = 1.0
    rope[0, :, CTX:] = np.cos(ang.astype(np.float32))
    rope[1, :, CTX:] = np.sin(ang.astype(np.float32)) * sign[:, None]
    return cst.astype(np.float32), rope.astype(np.float32)


def _na_bias_index():
    dr = np.zeros((5, 640, 128), np.int64)
    dc = np.zeros((5, 640, 128), np.int64)
    ok = np.zeros((5, 640, 128), bool)
    for ty, j in enumerate((0, 1, 2, 14, 15)):
        ks = min(max(2 * j - 4, 0), 22)
        qi = np.arange(128)
        r = 2 * j + qi // 64
        c = qi % 64
        rs = np.clip(r - 4, 0, 24)
        qwin = np.clip(c - 8, 0, 48)
        ki = np.arange(640)
        kr = ks + ki // 64
        kc = ki % 64
        v = (kr[:, None] >= rs[None, :]) & (kr[:, None] < rs[None, :] + 8) & \
            (kc[:, None] >= qwin[None, :]) & (kc[:, None] < qwin[None, :] + 16)
        ok[ty] = v
        dr[ty] = np.clip(kr[:, None] - r[None, :] + 7, 0, 14)
        dc[ty] = np.clip(kc[:, None] - c[None, :], -15, 15) + 15
    return dr, dc, ok


def _prep_shared(inp):
    L_ = np.asarray(inp['w_mod']).shape[0]
    f = lambda a: np.ascontiguousarray(np.asarray(a, dtype=np.float32))
    sh = {}
    for k in ("w_mod", "b_mod", "w_in", "w_branch", "w_out", "w1", "w3", "w2"):
        sh[k] = f(inp[k])
    g1 = f(inp["norm1_g"]).reshape(L_, 16, 128).transpose(0, 2, 1)
    g2 = f(inp["norm2_g"]).reshape(L_, 16, 128).transpose(0, 2, 1)
    sh["gT"] = f(np.stack([g1, g2], 1))
    pv = np.zeros((L_, 128, NPV), np.float32)
    pv[:, :, 0] = inp["na_q_g"]
    pv[:, :, 1] = inp["na_k_g"]
    pv[:, :, 2] = np.concatenate([inp["df_q_g"], inp["df_q_g"]], 1)
    pv[:, :, 3] = np.concatenate([inp["df_k_g"], inp["df_k_g"]], 1)
    pv[:, :, 4] = inp["df_sub_g"]
    pv[:, :, 8:40] = np.asarray(inp["rg_conv_w"]).reshape(L_, 4, 8, 128).transpose(0, 3, 1, 2).reshape(L_, 128, 32)
    pv[:, :, 40:48] = np.asarray(inp["rg_conv_b"]).reshape(L_, 8, 128).transpose(0, 2, 1)
    pv[:, :, 48:64] = np.asarray(inp["rg_b_a"]).reshape(L_, 2, 8, 128).transpose(0, 3, 1, 2).reshape(L_, 128, 16)
    pv[:, :, 64:80] = np.asarray(inp["rg_b_x"]).reshape(L_, 2, 8, 128).transpose(0, 3, 1, 2).reshape(L_, 128, 16)
    pv[:, :, 80:96] = np.asarray(inp["rg_lam"]).reshape(L_, 2, 8, 128).transpose(0, 3, 1, 2).reshape(L_, 128, 16)
    sh["pv"] = pv
    sh["df_lam"] = f(inp["df_lam"]).reshape(L_, 256)
    dr, dc, ok = _na_bias_index()
    rpb = f(inp["na_rpb"])
    tab = np.where(ok[None, None], rpb[:, :, dr, dc], np.float32(NEGB)).astype(np.float32)
    tab = tab.reshape(L_, 4, 5, 5, 128, 128).transpose(0, 1, 4, 2, 3, 5)
    sh["nab"] = f(tab.reshape(L_, 4, 128, 5 * 5 * 128))
    wa = f(inp["rg_w_a"]); wx = f(inp["rg_w_x"])
    rg = np.stack([wa, wx], 2)
    sh["rgw"] = f(rg.transpose(0, 3, 4, 1, 2, 5).reshape(L_, 8, 128, 4 * 128))
    wr = np.concatenate([f(inp["w_router_g"]), f(inp["w_router_e"])], 2)
    sh["wr"] = f(wr.reshape(L_, 16, 128, 36).transpose(0, 2, 1, 3).reshape(L_, 128, 16 * 36))
    sh["br"] = f(np.concatenate([f(inp["b_router_g"]), f(inp["b_router_e"])], 1))
    lc = np.zeros((L_, 128, 2), np.float32)
    for l in range(L_):
        li = 0.8 - 0.6 * float(np.exp(-0.3 * l))
        lc[l, :, 0] = -li
        lc[l, :, 1] = 1.0 - li
    sh["lcst"] = lc
    cst, rope = _host_consts()
    sh["cst"] = cst
    sh["rope"] = rope
    return sh


def _core_inputs(inp, sh, core):
    b0 = core * NB
    xin = np.concatenate([np.asarray(inp["ctx"][b0:b0 + NB], np.float32), np.asarray(inp["x"][b0:b0 + NB], np.float32)], axis=1)
    cv = np.stack([np.asarray(inp["c"][b0]), np.asarray(inp["c"][b0 + 1]), np.asarray(inp["c_ctx"])], 0).astype(np.float32)
    cvT = np.ascontiguousarray(cv.reshape(3, 16, 128).transpose(2, 1, 0))
    m = dict(sh)
    m["xin"] = np.ascontiguousarray(xin)
    m["cvecT"] = cvT
    return m


LAYERED = ("w_mod", "b_mod", "w_in", "w_branch", "w_out", "w1", "w3", "w2", "gT", "pv", "df_lam", "nab",
           "rgw", "wr", "br", "lcst")


def kernel(**inputs):
    n_cores = 8
    sh = _prep_shared(inputs)
    n_l = sh["w_mod"].shape[0]
    if DEBUG.get("fused"):
        nc = build_program(n_l, 99, ())
        in_maps = [_core_inputs(inputs, sh, c) for c in range(n_cores)]
        res = run_bass_kernel_spmd(nc, in_maps, core_ids=list(range(n_cores)))
        outs = [np.asarray(r["xout"])[:, CTX:, :] for r in res.results]
        return np.ascontiguousarray(np.concatenate(outs, axis=0).astype(np.float32))
    nc = build_program(1, 99, (), LW=1)
    base = [_core_inputs(inputs, {}, c) for c in range(n_cores)]
    xs = [m["xin"] for m in base]
    for l in range(n_l):
        in_maps = []
        for c in range(n_cores):
            m = {k: np.ascontiguousarray(sh[k][l:l + 1]) for k in LAYERED}
            m["cst"] = sh["cst"]
            m["rope"] = sh["rope"]
            m["xin"] = xs[c]
            m["cvecT"] = base[c]["cvecT"]
            in_maps.append(m)
        res = run_bass_kernel_spmd(nc, in_maps, core_ids=list(range(n_cores)))
        xs = [np.ascontiguousarray(np.asarray(r["xout"])) for r in res.results]
    outs = [x[:, CTX:, :] for x in xs]
    return np.ascontiguousarray(np.concatenate(outs, axis=0).astype(np.float32))
```

```python
import numpy as np
import concourse.bass as bass
import concourse.mybir as mybir
from concourse.bass_utils import run_bass_kernel_spmd
from contextlib import ExitStack

F32 = mybir.dt.float32
BF16 = mybir.dt.bfloat16
AF = mybir.ActivationFunctionType
ALU = mybir.AluOpType

L_ = 4
D = 2048
SEQ = 2048
CTX = 256
T = SEQ + CTX
NT = T // 128
NB = 2
EPS = 1e-6
NEGB = -30000.0
MIXC = 5120
INC = 11264
NPV = 96

G = {}
DEBUG = {}
PEW = "dve"

ENGS = ("pe", "act", "dve", "pool", "sp")
NSLOT = {"sp": 12, "pool": 8}


class Buf:
    __slots__ = ("w", "r", "excl")

    def __init__(self, excl=False):
        self.w = None
        self.r = {}
        self.excl = excl


def bufs(n):
    return [Buf() for _ in range(n)]


class Prog:
    def __init__(self, nc):
        self.nc = nc
        self.es = ExitStack()
        self.ops = {e: [] for e in ENGS}
        self.cnt = {e: 0 for e in ENGS}
        self.seen = {e: {} for e in ENGS}
        self.dcnt = {q: 0 for q in NSLOT}
        self.sems = {}
        for e in ("pe", "act", "dve", "pool"):
            self.sems[e] = self.es.enter_context(nc.semaphore("p_" + e))
        for q, n in NSLOT.items():
            for i in range(n):
                self.sems[(q, i)] = self.es.enter_context(nc.semaphore("d_%s%d" % (q, i)))

    def sb(self, name, shape, dtype=F32):
        return self.es.enter_context(self.nc.sbuf_tensor(name, list(shape), dtype))

    def ps(self, name, shape, dtype=F32):
        return self.es.enter_context(self.nc.psum_tensor(name, list(shape), dtype))

    def _waits(self, eng, reads, writes, extra=()):
        deps = {}

        def add(k, v):
            if deps.get(k, 0) < v:
                deps[k] = v
        for b in reads:
            if b.w is not None:
                add(*b.w)
            if b.excl:
                for k, v in b.r.items():
                    add(k, v)
        for b in writes:
            if b.w is not None:
                add(*b.w)
            for k, v in b.r.items():
                add(k, v)
        for k, v in extra:
            add(k, v)
        out = []
        seen = self.seen[eng]
        for k, v in deps.items():
            if k == eng and eng == "pe":
                continue
            if seen.get(k, 0) >= v:
                continue
            seen[k] = v
            out.append((k, v))
        return out

    def _mark(self, tok, reads, writes):
        k, v = tok
        for b in reads:
            if b.r.get(k, 0) < v:
                b.r[k] = v
        for b in writes:
            b.w = tok
            b.r = {}

    def op(self, eng, fn, reads=(), writes=()):
        waits = self._waits(eng, reads, writes)
        self.cnt[eng] += 1
        tok = (eng, self.cnt[eng])
        self.ops[eng].append((waits, fn, eng, 1))
        self._mark(tok, reads, writes)
        return tok

    def dma(self, q, fn, reads=(), writes=()):
        n = self.dcnt[q]
        ns = NSLOT[q]
        slot = (q, n % ns)
        prev = 16 * (n // ns)
        extra = [(slot, prev)] if prev > 0 else []
        waits = self._waits(q, reads, writes, extra)
        self.dcnt[q] += 1
        tok = (slot, prev + 16)
        self.ops[q].append((waits, fn, slot, 16))
        self._mark(tok, reads, writes)
        return tok

    def all_tokens(self):
        toks = [(e, self.cnt[e]) for e in ("pe", "act", "dve", "pool") if self.cnt[e] > 0]
        for q, ns in NSLOT.items():
            n = self.dcnt[q]
            for s in range(ns):
                if n > s:
                    toks.append(((q, s), 16 * ((n - 1 - s) // ns + 1)))
        return toks

    def fence(self):
        toks = self.all_tokens()
        for e in ENGS:
            waits = self._waits(e, (), (), toks)
            if waits:
                self.ops[e].append((waits, None, None, 0))

    def emit(self):
        nc = self.nc
        with nc.Block() as block:
            def run(name):
                def body(e):
                    for waits, fn, semk, inc in self.ops[name]:
                        for k, v in waits:
                            e.wait_ge(self.sems[k], v)
                        if fn is not None:
                            fn(e).then_inc(self.sems[semk], inc)
                return body
            block.tensor(run("pe"))
            block.scalar(run("act"))
            block.vector(run("dve"))
            block.gpsimd(run("pool"))
            block.sync(run("sp"))
        self.es.close()

    def mm(self, out, lhsT, rhs, start, stop, R, W):
        return self.op("pe", lambda e: e.matmul(out, lhsT, rhs, start=start, stop=stop), R, W)

    def tr(self, out, in_, ident, R, W):
        return self.op("pe", lambda e: e.transpose(out, in_, ident), R, W)

    def act(self, out, in_, func, R, W, scale=1.0, bias=0.0, accum=None):
        assert not isinstance(bias, float) or bias == 0.0, "float bias is mis-encoded as a pointer; pass an AP"
        if accum is None:
            return self.op("act", lambda e: e.activation(out=out, in_=in_, func=func, bias=bias, scale=scale), R, W)
        return self.op("act", lambda e: e.activation(out=out, in_=in_, func=func, bias=bias, scale=scale,
                                                     accum_out=accum), R, W)

    def ts(self, eng, out, in0, s1, s2, op0, op1, R, W):
        def f(e):
            if s2 is None:
                return e.tensor_scalar(out=out, in0=in0, scalar1=s1, scalar2=None, op0=op0)
            return e.tensor_scalar(out=out, in0=in0, scalar1=s1, scalar2=s2, op0=op0, op1=op1)
        return self.op(eng, f, R, W)

    def tt(self, eng, out, in0, in1, op, R, W):
        return self.op(eng, lambda e: e.tensor_tensor(out=out, in0=in0, in1=in1, op=op), R, W)

    def stt(self, out, in0, scalar, in1, op0, op1, R, W):
        return self.op("dve", lambda e: e.scalar_tensor_tensor(out=out, in0=in0, scalar=scalar, in1=in1,
                                                              op0=op0, op1=op1), R, W)

    def recip(self, out, in_, R, W):
        return self.op("dve", lambda e: e.reciprocal(out=out, in_=in_), R, W)

    def copy(self, eng, out, in_, R, W):
        return self.op(eng, lambda e: e.tensor_copy(out=out, in_=in_), R, W)

    def memset(self, eng, ap, val, W):
        return self.op(eng, lambda e: e.memset(ap, val), (), W)

    def scan(self, out, d0, d1, init, R, W):
        return self.op("dve", lambda e: e.tensor_tensor_scan(out=out, data0=d0, data1=d1, initial=init,
                                                            op0=ALU.mult, op1=ALU.add), R, W)

    def ld(self, q, out, in_, R, W):
        return self.dma(q, lambda e: e.dma_start(out=out, in_=in_), R, W)


class Arena:
    def __init__(self, P, words):
        self.t = P.sb("arena", [128, words], F32)
        self.words = words
        self.off = 0

    def reset(self):
        self.off = 0

    def _take(self, words):
        o = self.off
        self.off += words
        assert self.off <= self.words, ("arena overflow", self.off, self.words)
        return o

    def f32(self, *shape):
        n = int(np.prod(shape))
        o = self._take(n)
        ap = self.t[:, o:o + n]
        return self._shape(ap, shape)

    def bf16(self, *shape):
        n = int(np.prod(shape))
        w = (n + 1) // 2
        o = self._take(w)
        ap = self.t[:, o:o + w].bitcast(BF16)[:, 0:n]
        return self._shape(ap, shape)

    @staticmethod
    def _shape(ap, shape):
        if len(shape) == 1:
            return ap
        if len(shape) == 2:
            return ap.rearrange("p (a b) -> p a b", a=shape[0], b=shape[1])
        if len(shape) == 3:
            return ap.rearrange("p (a b c) -> p a b c", a=shape[0], b=shape[1], c=shape[2])
        raise ValueError(shape)


def who_of(tt):
    return 2 if (tt % NT) < 2 else tt // NT


def build_program(n_layers=L_, stages=99, dump=(), LW=L_):
    nc = bass.Bass("TRN2", target_bir_lowering=False)
    L_ = LW
    dt_in = lambda name, shape: nc.dram_tensor(name, list(shape), F32, kind="ExternalInput").ap()

    def scr(name, shape, dtype=F32):
        kind = "ExternalOutput" if name in dump else "Internal"
        return nc.dram_tensor(name, list(shape), dtype, kind=kind).ap()

    xin = dt_in("xin", [NB, T, D])
    cvecT = dt_in("cvecT", [128, 16, 3])
    w_mod = dt_in("w_mod", [L_, D, 6 * D])
    b_mod = dt_in("b_mod", [L_, 6 * D])
    gT_in = dt_in("gT", [L_, 2, 128, 16])
    w_in = dt_in("w_in", [L_, D, INC])
    pv_in = dt_in("pv", [L_, 128, NPV])
    dflam = dt_in("df_lam", [L_, 256])
    nab = dt_in("nab", [L_, 4, 128, 5 * 5 * 128])
    rgw = dt_in("rgw", [L_, 8, 128, 4 * 128])
    w_br = dt_in("w_branch", [L_, D, D])
    w_out = dt_in("w_out", [L_, D, D])
    wr_in = dt_in("wr", [L_, 128, 16 * 36])
    br_in = dt_in("br", [L_, 36])
    w1 = dt_in("w1", [L_, 32, D, 512])
    w3 = dt_in("w3", [L_, 32, D, 512])
    w2 = dt_in("w2", [L_, 32, 512, D])
    cst = dt_in("cst", [128, 384])
    lcst_in = dt_in("lcst", [L_, 128, 2])
    rope = dt_in("rope", [2, 128, T])
    xout = nc.dram_tensor("xout", [NB, T, D], F32, kind="ExternalOutput").ap()

    naK = scr("naK", [NB, 512, T], BF16)
    naQ = scr("naQ", [NB, 512, T], BF16)
    naV = scr("naV", [NB, T, 512], BF16)
    dfK = scr("dfK", [NB, 512, T], BF16)
    dfQ = scr("dfQ", [NB, 512, T], BF16)
    dfV = scr("dfV", [NB, T, 512], BF16)
    rgx = scr("rgx", [NB, 1024, T], F32)
    rgg = scr("rgg", [NB, 1024, T], F32)
    gts = scr("gts", [NB, 3 * D, T], BF16)
    yT = scr("yT", [NB, D, T], BF16)
    h2T = scr("h2T", [NB, D, T], BF16)
    wcm = scr("wcm", [NB * NT, 128, 32], F32)
    gtrow = scr("gtrow", [2, 3, D], F32)

    P = Prog(nc)
    AR = Arena(P, 47616)
    psb = [P.ps("psb%d" % i, [128, 512], F32) for i in range(8)]
    PB = [Buf(excl=True) for _ in range(8)]
    c_f = P.sb("c_f", [128, 384], F32)
    c_b = P.sb("c_b", [128, 384], BF16)
    ones_b = P.sb("ones_b", [128, 128], BF16)
    scT = P.sb("scT", [128, 16, 3], BF16)
    modT = P.sb("modT", [128, 96, 3], F32)
    S12 = P.sb("S12", [128, 2, 16, 3], F32)
    pvt = P.sb("pvt", [128, NPV], F32)
    lam_t = P.sb("lam_t", [128, 8], F32)
    gsub = P.sb("gsub", [128, 1], F32)
    rgc = P.sb("rgc", [128, 2, 16], F32)
    CB = Buf()
    cbt = P.sb("cbt", [128, 2], F32)
    G["eps"] = cbt[:, 0:1]
    G["one"] = cbt[:, 1:2]
    ident = c_f[:, 0:128]
    rot_b = c_b[:, 128:256]
    blk_b = c_b[:, 256:384]

    P.ld("sp", c_f[:], cst, [], [CB])
    P.copy("dve", c_b[:], c_f[:], [CB], [CB])
    P.memset("dve", ones_b[:], 1.0, [CB])
    P.memset("dve", cbt[:, 0:1], EPS, [CB])
    P.memset("dve", cbt[:, 1:2], 1.0, [CB])
    AR.reset()
    cv = AR.f32(16, 3)
    cs = AR.f32(16, 3)
    TB = Buf()
    P.ld("sp", cv, cvecT, [], [TB])
    P.act(cs, cv, AF.Sigmoid, [TB], [TB])
    P.tt("dve", scT[:], cv, cs, ALU.mult, [TB], [CB])
    P.fence()

    for l in range(n_layers):
        lam_init = 0.8 - 0.6 * float(np.exp(-0.3 * l))
        xsrc = xin if l == 0 else xout

        AR.reset()
        MB = Buf()
        bmod_b = AR.bf16(6 * D)
        wsl = [AR.bf16(16, 512) for _ in range(2)]
        WS = bufs(2)
        g12 = AR.f32(2, 16)
        gsb = [AR.f32(512) for _ in range(2)]
        GS = bufs(2)
        dl = AR.f32(256)
        dtmp = AR.f32(64)
        P.ld("pool", bmod_b[0:1, :].rearrange("o (a b) -> o a b", b=512), b_mod[l:l + 1, :].rearrange("o (a b) -> o a b", b=512), [], [MB])
        P.ld("sp", g12, gT_in[l].rearrange("j p c -> p j c"), [], [MB])
        P.ld("sp", pvt[:], pv_in[l], [], [MB])
        P.ld("sp", dl, dflam[l:l + 1, :].broadcast_to([128, 256]), [], [MB])
        lct = AR.f32(2)
        P.ld("sp", lct, lcst_in[l], [], [MB])
        psM = psb[0]
        psG = [psb[1], psb[2]]
        ng = 0
        for s in range(24):
            wb = wsl[s % 2]
            P.ld("pool", wb, w_mod[l][:, s * 512:(s + 1) * 512].rearrange("(c p) n -> p c n", p=128), [], [WS[s % 2]])
            for cc in range(4):
                ch = s * 4 + cc
                for kc in range(16):
                    P.mm(psM[:, ch * 3:ch * 3 + 3], wb[:, kc, cc * 128:(cc + 1) * 128], scT[:, kc, :],
                         kc == 0, False, [WS[s % 2], CB], [PB[0]])
                P.mm(psM[:, ch * 3:ch * 3 + 3], bmod_b[0:1, ch * 128:(ch + 1) * 128], ones_b[0:1, 0:3],
                     False, True, [MB, CB], [PB[0]])
            if s // 4 in (2, 5):
                j = 0 if s // 4 == 2 else 1
                pg = psG[ng % 2]
                for kc in range(16):
                    P.mm(pg[0:3, :], scT[:, kc, :], wb[:, kc, :], kc == 0, False, [WS[s % 2], CB], [PB[1 + ng % 2]])
                P.mm(pg[0:3, :], ones_b[0:1, 0:3], bmod_b[0:1, s * 512:(s + 1) * 512], False, True,
                     [MB, CB], [PB[1 + ng % 2]])
                P.act(gsb[ng % 2][0:3, :], pg[0:3, :], AF.Copy, [PB[1 + ng % 2]], [GS[ng % 2]])
                P.ld("sp", gtrow[j, :, (s % 4) * 512:(s % 4 + 1) * 512], gsb[ng % 2][0:3, :], [GS[ng % 2]], [])
                ng += 1
        P.act(modT[:].rearrange("p a b -> p (a b)"), psM[:, 0:288], AF.Copy, [PB[0]], [MB])
        for j in range(2):
            for w in range(3):
                P.stt(S12[:, j, :, w], modT[:, (1 + 3 * j) * 16:(2 + 3 * j) * 16, w], 1.0, g12[:, j, :],
                      ALU.add, ALU.mult, [MB], [MB])
        P.tt("dve", dtmp, dl[:, 0:64], dl[:, 64:128], ALU.mult, [MB], [MB])
        P.op("dve", lambda e, o=lam_t[:, 2:3], i=dtmp: e.reduce_sum(out=o, in_=i, axis=mybir.AxisListType.X), [MB], [MB])
        P.tt("dve", dtmp, dl[:, 128:192], dl[:, 192:256], ALU.mult, [MB], [MB])
        P.op("dve", lambda e, o=lam_t[:, 3:4], i=dtmp: e.reduce_sum(out=o, in_=i, axis=mybir.AxisListType.X), [MB], [MB])
        P.act(lam_t[:, 4:6], lam_t[:, 2:4], AF.Exp, [MB], [MB])
        P.tt("dve", lam_t[:, 6:7], lam_t[:, 5:6], lam_t[:, 4:5], ALU.subtract, [MB], [MB])
        P.ts("dve", lam_t[:, 0:1], lam_t[:, 6:7], lct[:, 0:1], None, ALU.add, None, [MB], [MB])
        P.ts("dve", gsub[:], pvt[:, 4:5], lct[:, 1:2], None, ALU.mult, None, [MB], [MB])
        P.act(rgc[:, 0, :], pvt[:, 80:96], AF.Exp, [MB], [MB], scale=-1.0)
        P.act(rgc[:, 0, :], rgc[:, 0, :], AF.Ln, [MB], [MB], bias=G['one'])
        P.ts("dve", rgc[:, 1, :], rgc[:, 0, :], -16.0, None, ALU.mult, None, [MB], [MB])
        P.ts("dve", rgc[:, 0, :], rgc[:, 0, :], -8.0, None, ALU.mult, None, [MB], [MB])
        P.fence()
        if stages < 1:
            break

        AR.reset()
        cos_t = AR.f32(T)
        sin_t = AR.f32(T)
        RB = Buf()
        P.ld("sp", cos_t, rope[0], [], [RB])
        P.ld("sp", sin_t, rope[1], [], [RB])
        hT = AR.bf16(16, 768)
        HT = bufs(6)
        xt = [AR.f32(D) for _ in range(2)]
        XT = bufs(2)
        st = [AR.f32(4) for _ in range(2)]
        ST = bufs(2)
        wsl = [AR.bf16(16, 512) for _ in range(2)]
        WS = bufs(2)
        ev = [dict(sq=AR.bf16(384), zc=AR.f32(384), zg=AR.bf16(384), t1=AR.f32(384), t2=AR.f32(384),
                   rs=AR.f32(384), o16=AR.bf16(384), o32=AR.f32(384)) for _ in range(2)]
        EV = [dict(sq=Buf(), zc=Buf(), zg=Buf(), t1=Buf(), t2=Buf(), rs=Buf(), o16=Buf(), o32=Buf()) for _ in range(2)]
        vo = [AR.bf16(512) for _ in range(2)]
        VO = bufs(2)
        junk = AR.f32(D); JK = Buf()
        nld = 0
        nev = 0
        nvo = 0
        nx = 0
        for blk in range(DEBUG.get('pblks', 6)):
            b = blk // 3
            t0 = (blk % 3) * 768
            for i in range(6):
                tt_ = blk * 6 + i
                w = who_of(tt_)
                x_ = xt[nx % 2]; X_ = XT[nx % 2]; s_ = st[nx % 2]; S_ = ST[nx % 2]
                nx += 1
                P.ld("sp", x_, xsrc[b, t0 + i * 128:t0 + (i + 1) * 128, :], [], [X_])
                norm_mod_T(P, x_, X_, s_, S_, psb, PB, ident, CB, S12[:, 0, :, w], modT[:, 0:16, w], MB,
                           hT[:, :, i * 128:(i + 1) * 128], HT[i], junk, JK)
            for s in DEBUG.get('pslabs', range(22)):
                wb = wsl[nld % 2]; WB = WS[nld % 2]
                nld += 1
                P.ld("pool", wb, w_in[l][:, s * 512:(s + 1) * 512].rearrange("(c p) n -> p c n", p=128), [], [WB])
                if s in (1, 3):
                    dst = naV if s == 1 else dfV
                    for i in range(6):
                        pb_i = 6 + (nvo % 2)
                        for kc in range(16):
                            P.mm(psb[pb_i][:, :], hT[:, kc, i * 128:(i + 1) * 128], wb[:, kc, :], kc == 0, kc == 15,
                                 [HT[i], WB], [PB[pb_i]])
                        P.act(vo[nvo % 2], psb[pb_i][:, :], AF.Copy, [PB[pb_i]], [VO[nvo % 2]])
                        P.ld("sp", dst[b, t0 + i * 128:t0 + (i + 1) * 128, :], vo[nvo % 2], [VO[nvo % 2]], [])
                        nvo += 1
                    continue
                for cc in range(4):
                    for hf in range(2):
                        pi = nev % 2
                        E = ev[pi]; EB = EV[pi]
                        nev += 1
                        pz = psb[pi]
                        tk = slice(t0 + hf * 384, t0 + (hf + 1) * 384)
                        for kc in range(16):
                            P.mm(pz[:, 0:384], wb[:, kc, cc * 128:(cc + 1) * 128], hT[:, kc, hf * 384:(hf + 1) * 384],
                                 kc == 0, kc == 15, HT[hf * 3:hf * 3 + 3] + [WB], [PB[pi]])
                        if s in (0, 6):
                            dst = naK if s == 0 else naQ
                            gcol = pvt[:, 1:2] if s == 0 else pvt[:, 0:1]
                            P.act(E["sq"], pz[:, 0:384], AF.Square, [PB[pi]], [EB["sq"]])
                            P.ts("dve", E["zc"], pz[:, 0:384], gcol, None, ALU.mult, None, [PB[pi], MB], [EB["zc"]])
                            pn = psb[2 + pi]
                            P.mm(pn[:, 0:384], ones_b[:], E["sq"], True, True, [EB["sq"], CB], [PB[2 + pi]])
                            P.act(E["rs"], pn[:, 0:384], AF.Sqrt, [PB[2 + pi]], [EB["rs"]], scale=1.0 / 128, bias=G['eps'])
                            P.recip(E["rs"], E["rs"], [EB["rs"]], [EB["rs"]])
                            P.tt(PEW, E["o16"], E["zc"], E["rs"], ALU.mult, [EB["zc"], EB["rs"]], [EB["o16"]])
                            P.ld("sp", dst[b, cc * 128:(cc + 1) * 128, tk], E["o16"], [EB["o16"]], [])
                        elif s in (2, 7):
                            dst = dfK if s == 2 else dfQ
                            gcol = pvt[:, 3:4] if s == 2 else pvt[:, 2:3]
                            P.act(E["sq"], pz[:, 0:384], AF.Square, [PB[pi]], [EB["sq"]])
                            P.ts("dve", E["zg"], pz[:, 0:384], gcol, None, ALU.mult, None, [PB[pi], MB], [EB["zg"]])
                            pn = psb[2 + pi]
                            pr = psb[4 + pi]
                            P.mm(pn[:, 0:384], blk_b, E["sq"], True, True, [EB["sq"], CB], [PB[2 + pi]])
                            dfm = DEBUG.get("dfmode", 0)
                            if dfm < 2:
                                P.mm(pr[:, 0:384], rot_b, E["zg"], True, True, [EB["zg"], CB], [PB[4 + pi]])
                            P.tt(PEW, E["t1"], E["zg"], cos_t[:, tk], ALU.mult, [EB["zg"], RB], [EB["t1"]])
                            if dfm < 1:
                                P.tt("dve", E["t2"], pr[:, 0:384], sin_t[:, tk], ALU.mult, [PB[4 + pi], RB], [EB["t2"]])
                                P.tt(PEW, E["t1"], E["t1"], E["t2"], ALU.add, [EB["t1"], EB["t2"]], [EB["t1"]])
                            P.act(E["rs"], pn[:, 0:384], AF.Sqrt, [PB[2 + pi]], [EB["rs"]], scale=1.0 / 64, bias=G['eps'])
                            P.recip(E["rs"], E["rs"], [EB["rs"]], [EB["rs"]])
                            P.tt("dve", E["o16"], E["t1"], E["rs"], ALU.mult, [EB["t1"], EB["rs"]], [EB["o16"]])
                            P.ld("sp", dst[b, cc * 128:(cc + 1) * 128, tk], E["o16"], [EB["o16"]], [])
                            if DEBUG.get("ufence"):
                                P.fence()
                        elif s in (4, 5, 8, 9):
                            dst = rgx if s in (4, 5) else rgg
                            r0 = ((s - 4) % 4) * 512 + cc * 128 if s in (4, 5) else (s - 8) * 512 + cc * 128
                            P.act(E["o32"], pz[:, 0:384], AF.Copy, [PB[pi]], [EB["o32"]])
                            P.ld("sp", dst[b, r0:r0 + 128, tk], E["o32"], [EB["o32"]], [])
                        else:
                            r0 = (s - 10) * 512 + cc * 128
                            P.act(E["o16"], pz[:, 0:384], AF.Sigmoid, [PB[pi]], [EB["o16"]])
                            P.ld("sp", gts[b, r0:r0 + 128, tk], E["o16"], [EB["o16"]], [])
        P.fence()
        if stages < 2:
            break

        na_scale = 128.0 ** -0.5
        for b in range(NB):
            for h in range(4):
                AR.reset()
                kT = AR.bf16(T); qT = AR.bf16(T); V = AR.bf16(NT, 128); bia = AR.f32(5, 5, 128)
                ya = AR.bf16(T)
                LB = Buf(); YA = Buf()
                P.ld("sp", kT, naK[b, h * 128:(h + 1) * 128, :], [], [LB])
                P.ld("sp", qT, naQ[b, h * 128:(h + 1) * 128, :], [], [LB])
                P.ld("sp", V, naV[b, :, h * 128:(h + 1) * 128].rearrange("(t p) e -> p t e", p=128), [], [LB])
                P.ld("sp", bia.rearrange("p a b c -> p (a b c)"), nab[l, h], [], [LB])
                tmp = [AR.f32(640) for _ in range(2)]; TM = bufs(2)
                PT = [AR.bf16(896) for _ in range(2)]; PTB = bufs(2)
                rsm = [AR.f32(128) for _ in range(2)]; RS = bufs(2)
                for qi in range(NT):
                    pi = qi % 2
                    qc = slice(qi * 128, (qi + 1) * 128)
                    pA, pB_, pO, pS = psb[pi * 4], psb[pi * 4 + 1], psb[pi * 4 + 2], psb[pi * 4 + 3]
                    BA, BB, BO, BS = PB[pi * 4], PB[pi * 4 + 1], PB[pi * 4 + 2], PB[pi * 4 + 3]
                    if qi < 2:
                        for c in range(2):
                            P.mm(pA[:, c * 128:(c + 1) * 128], kT[:, c * 128:(c + 1) * 128], qT[:, qc], True, True, [LB], [BA])
                        P.act(PT[pi][:, 0:256], pA[:, 0:256], AF.Exp, [BA], [PTB[pi]], scale=na_scale)
                        vts = [0, 1]
                    else:
                        j = qi - 2
                        ks = min(max(2 * j - 4, 0), 22)
                        ty = 0 if j == 0 else 1 if j == 1 else 3 if j == 14 else 4 if j == 15 else 2
                        k0 = 256 + ks * 64
                        for c in range(5):
                            tgt = pA[:, c * 128:(c + 1) * 128] if c < 4 else pB_[:, 0:128]
                            P.mm(tgt, kT[:, k0 + c * 128:k0 + (c + 1) * 128], qT[:, qc], True, True, [LB], [BA if c < 4 else BB])
                        for c in range(2):
                            P.mm(pB_[:, (1 + c) * 128:(2 + c) * 128], kT[:, c * 128:(c + 1) * 128], qT[:, qc], True, True, [LB], [BB])
                        P.stt(tmp[pi][:, 0:512], pA[:, 0:512], na_scale, bia[:, ty, 0:4, :].rearrange("p a b -> p (a b)"),
                              ALU.mult, ALU.add, [BA, LB], [TM[pi]])
                        P.stt(tmp[pi][:, 512:640], pB_[:, 0:128], na_scale, bia[:, ty, 4, :], ALU.mult, ALU.add, [BB, LB], [TM[pi]])
                        P.act(PT[pi][:, 0:640], tmp[pi][:, 0:640], AF.Exp, [TM[pi]], [PTB[pi]])
                        P.act(PT[pi][:, 640:896], pB_[:, 128:384], AF.Exp, [BB], [PTB[pi]], scale=na_scale)
                        vts = [2 + ks // 2 + c for c in range(5)] + [0, 1]
                    n = len(vts)
                    for c, vt in enumerate(vts):
                        P.mm(pO[:, 0:128], V[:, vt, :], PT[pi][:, c * 128:(c + 1) * 128], c == 0, c == n - 1, [LB, PTB[pi]], [BO])
                    for c, vt in enumerate(vts):
                        P.mm(pS[:, 0:128], ones_b[:], PT[pi][:, c * 128:(c + 1) * 128], c == 0, c == n - 1, [CB, PTB[pi]], [BS])
                    P.recip(rsm[pi], pS[:, 0:128], [BS], [RS[pi]])
                    P.tt("dve", ya[:, qc], pO[:, 0:128], rsm[pi], ALU.mult, [BO, RS[pi]], [YA])
                P.ld("sp", yT[b, h * 128:(h + 1) * 128, :], ya, [YA], [])
                P.fence()
        if stages < 3:
            break

        for b in range(NB):
            for h in range(4):
                AR.reset()
                kT = [AR.bf16(T) for _ in range(2)]; qT = [AR.bf16(T) for _ in range(2)]
                V = AR.bf16(NT, 128); yb = AR.bf16(T)
                LB = Buf(); YB = Buf()
                for m in range(2):
                    r0 = h * 128 + m * 64
                    P.ld("sp", kT[m][0:64, :], dfK[b, r0:r0 + 64, :], [], [LB])
                    P.ld("sp", qT[m][0:64, :], dfQ[b, r0:r0 + 64, :], [], [LB])
                P.ld("sp", V, dfV[b, :, h * 128:(h + 1) * 128].rearrange("(t p) e -> p t e", p=128), [], [LB])
                PT = [AR.bf16(512) for _ in range(3)]; PTB = bufs(3)
                r_ = [AR.f32(512) for _ in range(2)]; RB_ = bufs(2)
                t_ = [AR.f32(512) for _ in range(2)]; TB_ = bufs(2)
                o_ = AR.f32(512); OB = Buf()
                sq = AR.bf16(512); SQ = Buf()
                rs = AR.f32(512); RSB = Buf()
                npt = 0
                for qb in range(5):
                    if qb == 0:
                        q0, N, kcs = 0, 256, [0, 1]
                    else:
                        q0, N, kcs = 256 + (qb - 1) * 512, 512, list(range(NT))
                    for m in range(2):
                        pO, pS = psb[3 + 2 * m], psb[4 + 2 * m]
                        BO, BS = PB[3 + 2 * m], PB[4 + 2 * m]
                        for ci, kc in enumerate(kcs):
                            pi = npt % 3
                            npt += 1
                            P.mm(psb[pi][:, 0:N], kT[m][0:64, kc * 128:(kc + 1) * 128], qT[m][0:64, q0:q0 + N], True, True, [LB], [PB[pi]])
                            P.act(PT[pi][:, 0:N], psb[pi][:, 0:N], AF.Exp, [PB[pi]], [PTB[pi]], scale=0.125)
                            P.mm(pO[:, 0:N], V[:, kc, :], PT[pi][:, 0:N], ci == 0, ci == len(kcs) - 1, [LB, PTB[pi]], [BO])
                            P.mm(pS[:, 0:N], ones_b[:], PT[pi][:, 0:N], ci == 0, ci == len(kcs) - 1, [CB, PTB[pi]], [BS])
                        P.recip(r_[m][:, 0:N], pS[:, 0:N], [BS], [RB_[m]])
                        P.tt("dve", t_[m][:, 0:N], pO[:, 0:N], r_[m][:, 0:N], ALU.mult, [BO, RB_[m]], [TB_[m]])
                    P.stt(o_[:, 0:N], t_[1][:, 0:N], lam_t[:, 0:1], t_[0][:, 0:N], ALU.mult, ALU.add, [TB_[0], TB_[1], MB], [OB])
                    P.act(sq[:, 0:N], o_[:, 0:N], AF.Square, [OB], [SQ])
                    P.mm(psb[7][:, 0:N], ones_b[:], sq[:, 0:N], True, True, [SQ, CB], [PB[7]])
                    P.act(rs[:, 0:N], psb[7][:, 0:N], AF.Sqrt, [PB[7]], [RSB], scale=1.0 / 128, bias=G['eps'])
                    P.recip(rs[:, 0:N], rs[:, 0:N], [RSB], [RSB])
                    P.stt(yb[:, q0:q0 + N], o_[:, 0:N], gsub[:, 0:1], rs[:, 0:N], ALU.mult, ALU.mult, [OB, RSB, MB], [YB])
                P.ld("sp", yT[b, 512 + h * 128:512 + (h + 1) * 128, :], yb, [YB], [])
                P.fence()
        if stages < 4:
            break

        for b in range(NB):
            for n in range(8):
                AR.reset()
                zc = AR.f32(CTX + 3); zl = AR.f32(SEQ + 3)
                ZB = Buf()
                wrg = AR.bf16(4, 128); WB = Buf()
                u = AR.f32(T); UB = Buf()
                ub = AR.bf16(T); UBB = Buf()
                gate = AR.f32(T); GB = Buf()
                r_ = AR.f32(T); RB_ = Buf()
                i_ = AR.f32(T); IB = Buf()
                a_ = AR.f32(T); AB = Buf()
                m_ = AR.f32(T); MB2 = Buf()
                hs = [AR.f32(T) for _ in range(2)]; HB = bufs(2)
                yc = AR.bf16(T); YC = Buf()
                P.memset(PEW, zc, 0.0, [ZB])
                P.memset(PEW, zl, 0.0, [ZB])
                P.ld("sp", zc[:, 2:2 + CTX], rgx[b, n * 128:(n + 1) * 128, 0:CTX], [], [ZB])
                P.ld("sp", zl[:, 2:2 + SEQ], rgx[b, n * 128:(n + 1) * 128, CTX:T], [], [ZB])
                P.ld("sp", gate, rgg[b, n * 128:(n + 1) * 128, :], [], [GB])
                P.ld("pool", wrg.rearrange("p a b -> p (a b)"), rgw[l, n], [], [WB])
                for (z, o0, N) in ((zc, 0, CTX), (zl, CTX, SEQ)):
                    P.ts("dve", u[:, o0:o0 + N], z[:, 0:N], pvt[:, 8 + n:9 + n], pvt[:, 40 + n:41 + n], ALU.mult, ALU.add, [ZB, MB], [UB])
                    for jj in range(1, 4):
                        P.stt(u[:, o0:o0 + N], z[:, jj:jj + N], pvt[:, 8 + jj * 8 + n:9 + jj * 8 + n], u[:, o0:o0 + N],
                              ALU.mult, ALU.add, [ZB, MB], [UB])
                P.copy(PEW, ub, u, [UB], [UBB])
                for d in range(2):
                    for (wi, dstt, DB, bcol) in ((0, r_, RB_, 48 + d * 8 + n), (1, i_, IB, 64 + d * 8 + n)):
                        for cblk in range(5):
                            c0 = cblk * 512
                            N = min(512, T - c0)
                            pi = (cblk + wi) % 2
                            P.mm(psb[pi][:, 0:N], wrg[:, d * 2 + wi, :], ub[:, c0:c0 + N], True, True, [WB, UBB], [PB[pi]])
                            P.act(dstt[:, c0:c0 + N], psb[pi][:, 0:N], AF.Sigmoid, [PB[pi], MB], [DB], bias=pvt[:, bcol:bcol + 1])
                    P.act(a_, r_, AF.Exp, [RB_, MB], [AB], scale=rgc[:, 0, d * 8 + n:d * 8 + n + 1])
                    P.act(m_, r_, AF.Exp, [RB_, MB], [MB2], scale=rgc[:, 1, d * 8 + n:d * 8 + n + 1])
                    P.ts("dve", m_, m_, -1.0, 1.0, ALU.mult, ALU.add, [MB2], [MB2])
                    P.act(m_, m_, AF.Sqrt, [MB2], [MB2])
                    P.tt(PEW, i_, i_, u, ALU.mult, [IB, UB], [IB])
                    P.tt("dve", m_, m_, i_, ALU.mult, [MB2, IB], [MB2])
                    if d == 0:
                        P.scan(hs[0], a_, m_, 0.0, [AB, MB2], [HB[0]])
                    else:
                        P.scan(hs[1][:, 0:CTX][:, ::-1], a_[:, 0:CTX][:, ::-1], m_[:, 0:CTX][:, ::-1], 0.0, [AB, MB2], [HB[1]])
                        P.scan(hs[1][:, CTX:T][:, ::-1], a_[:, CTX:T][:, ::-1], m_[:, CTX:T][:, ::-1], hs[1][:, 0:1],
                               [AB, MB2, HB[1]], [HB[1]])
                P.tt(PEW, hs[0], hs[0], hs[1], ALU.add, [HB[0], HB[1]], [HB[0]])
                P.act(r_, gate, AF.Square, [GB], [RB_])
                P.ts("dve", r_, r_, 0.044715, 1.0, ALU.mult, ALU.add, [RB_], [RB_])
                P.tt("dve", r_, r_, gate, ALU.mult, [RB_, GB], [RB_])
                P.act(r_, r_, AF.Sigmoid, [RB_], [RB_], scale=1.5957691216057308)
                P.tt(PEW, r_, r_, gate, ALU.mult, [RB_, GB], [RB_])
                P.tt("dve", yc, r_, hs[0], ALU.mult, [RB_, HB[0]], [YC])
                P.ld("sp", yT[b, 1024 + n * 128:1024 + (n + 1) * 128, :], yc, [YC], [])
                P.fence()
        if stages < 5:
            break

        AR.reset()
        gtb = AR.f32(3, D); GTB = Buf()
        P.ld("sp", gtb.rearrange("p a b -> p (a b)"), gtrow[0:1].rearrange("o w d -> o (w d)").broadcast_to([128, 3 * D]), [], [GTB])
        wr_t = AR.f32(16, 36); WRB = Buf()
        br_t = AR.f32(36)
        P.ld("sp", wr_t.rearrange("p a b -> p (a b)"), wr_in[l], [], [WRB])
        P.ld("sp", br_t, br_in[l:l + 1, :].broadcast_to([128, 36]), [], [WRB])
        ysb = AR.bf16(16, 768); YS = Buf()
        mT = AR.bf16(16, 768); MT = bufs(16)
        wsl = [AR.bf16(16, 512) for _ in range(2)]; WS = bufs(2)
        gsb3 = [AR.bf16(3, 768) for _ in range(2)]; G3 = bufs(2)
        ta = [AR.f32(384) for _ in range(2)]; TA = bufs(2)
        tb = [AR.f32(384) for _ in range(2)]; TBb = bufs(2)
        xt = [AR.f32(D) for _ in range(2)]; XT = bufs(2)
        junk = AR.f32(D); JK = Buf()
        st = [AR.f32(4) for _ in range(2)]; ST = bufs(2)
        h32 = [AR.f32(16, 128) for _ in range(2)]; H32 = bufs(2)
        h16 = [AR.bf16(16, 128) for _ in range(2)]; H16 = bufs(2)
        rt = [AR.f32(80) for _ in range(2)]; RT = bufs(2)
        nld = 0; ngl = 0; nu = 0; nx = 0
        for blk in range(6):
            b = blk // 3
            t0 = (blk % 3) * 768
            P.ld("sp", ysb, yT[b, :, t0:t0 + 768].rearrange("(c p) t -> p c t", p=128), [], [YS])
            for g in range(4):
                wb = wsl[nld % 2]; WB = WS[nld % 2]; nld += 1
                P.ld("pool", wb, w_br[l][:, g * 512:(g + 1) * 512].rearrange("(c p) n -> p c n", p=128), [], [WB])
                for cc in range(4):
                    dc = g * 4 + cc
                    g3 = gsb3[ngl % 2]; G3B = G3[ngl % 2]; ngl += 1
                    P.ld("sp", g3, gts[b, :, t0:t0 + 768].rearrange("(j r) t -> r j t", j=3)[dc * 128:(dc + 1) * 128], [], [G3B])
                    for hf in range(2):
                        pi = nu % 2; nu += 1
                        tk = slice(hf * 384, (hf + 1) * 384)
                        pa, pb2, pc = psb[pi * 3], psb[pi * 3 + 1], psb[pi * 3 + 2]
                        for (pp, BBp, k0, k1) in ((pa, PB[pi * 3], 0, 4), (pb2, PB[pi * 3 + 1], 4, 8), (pc, PB[pi * 3 + 2], 8, 16)):
                            for kc in range(k0, k1):
                                P.mm(pp[:, 0:384], wb[:, kc, cc * 128:(cc + 1) * 128], ysb[:, kc, tk], kc == k0, kc == k1 - 1, [WB, YS], [BBp])
                        P.tt("dve", ta[pi], pa[:, 0:384], g3[:, 0, tk], ALU.mult, [PB[pi * 3], G3B], [TA[pi]])
                        P.tt("dve", tb[pi], pb2[:, 0:384], g3[:, 1, tk], ALU.mult, [PB[pi * 3 + 1], G3B], [TBb[pi]])
                        P.tt(PEW, ta[pi], ta[pi], tb[pi], ALU.add, [TA[pi], TBb[pi]], [TA[pi]])
                        P.tt("dve", tb[pi], pc[:, 0:384], g3[:, 2, tk], ALU.mult, [PB[pi * 3 + 2], G3B, TA[pi]], [TBb[pi]])
                        P.tt(PEW, mT[:, dc, tk], ta[pi], tb[pi], ALU.add, [TA[pi], TBb[pi]], [MT[dc]])
            XO = bufs(6)
            for ps_ in range(2):
                wos = []
                for g in (2 * ps_, 2 * ps_ + 1):
                    wb = wsl[nld % 2]; WB = WS[nld % 2]; nld += 1
                    wos.append((g, wb, WB))
                    P.ld("pool", wb, w_out[l][:, g * 512:(g + 1) * 512].rearrange("(c p) n -> p c n", p=128), [], [WB])
                for i in range(6):
                    tt_ = blk * 6 + i
                    w = who_of(tt_)
                    x_ = xt[nx % 2]; X_ = XT[nx % 2]; nx += 1
                    hs_ = slice(ps_ * 1024, (ps_ + 1) * 1024)
                    P.ld("sp", x_[:, 0:1024], xsrc[b, t0 + i * 128:t0 + (i + 1) * 128, hs_], [XO[i]], [X_])
                    for (gg, wbb, WBB) in wos:
                        pi = 6 + (gg % 2)
                        for kc in range(16):
                            P.mm(psb[pi][:, :], mT[:, kc, i * 128:(i + 1) * 128], wbb[:, kc, :], kc == 0, kc == 15, MT + [WBB], [PB[pi]])
                        lo = (gg % 2) * 512
                        P.tt("dve", x_[:, 1024 + lo:1024 + lo + 512], psb[pi][:, :], gtb[:, w, gg * 512:(gg + 1) * 512], ALU.mult, [PB[pi], GTB], [X_])
                        P.tt(PEW, x_[:, lo:lo + 512], x_[:, lo:lo + 512], x_[:, 1024 + lo:1024 + lo + 512], ALU.add, [X_], [X_])
                    P.ld("sp", xout[b, t0 + i * 128:t0 + (i + 1) * 128, hs_], x_[:, 0:1024], [X_], [XO[i]])
            for i in range(6):
                tt_ = blk * 6 + i
                w = who_of(tt_)
                ix = i % 2
                x_ = xt[nx % 2]; X_ = XT[nx % 2]; nx += 1
                P.ld("sp", x_, xout[b, t0 + i * 128:t0 + (i + 1) * 128, :], [XO[i]], [X_])
                norm_mod_T(P, x_, X_, st[ix], ST[ix], psb, PB, ident, CB, S12[:, 1, :, w], modT[:, 48:64, w], MB,
                           h32[ix], H32[ix], junk, JK, banks=(0, 1, 2, 3))
                P.copy(PEW, h16[ix], h32[ix], [H32[ix]], [H16[ix]])
                P.ld("sp", h2T[b, :, t0 + i * 128:t0 + (i + 1) * 128].rearrange("(c p) t -> p c t", p=128), h16[ix], [H16[ix]], [])
                pr = psb[4 + ix]
                for kc in range(16):
                    P.mm(pr[:, 0:36], h32[ix][:, kc, :], wr_t[:, kc, :], kc == 0, kc == 15, [H32[ix], WRB], [PB[4 + ix]])
                route(P, pr[:, 0:36], PB[4 + ix], br_t, WRB, rt[ix], RT[ix])
                P.ld("sp", wcm[tt_], rt[ix][:, 0:32], [RT[ix]], [])
        P.fence()
        if stages < 6:
            break

        AR.reset()
        gtb = AR.f32(3, D); GTB = Buf()
        P.ld("sp", gtb.rearrange("p a b -> p (a b)"), gtrow[1:2].rearrange("o w d -> o (w d)").broadcast_to([128, 3 * D]), [], [GTB])
        hb = AR.bf16(16, 768); HBB = Buf()
        acc = AR.f32(6, D); ACC = bufs(6)
        wc = AR.f32(6, 32); WCB = Buf()
        w13 = [AR.bf16(16, 256) for _ in range(4)]; W13 = bufs(4)
        w2s = [AR.bf16(4, D) for _ in range(2)]; W2 = bufs(2)
        hid = AR.bf16(4, 768); HID = bufs(4)
        sl = [AR.f32(384) for _ in range(2)]; SL = bufs(2)
        xt = [AR.f32(D) for _ in range(2)]; XT = bufs(2)
        n13 = 0; n2 = 0; nu = 0; nx = 0
        last = False
        for blk in range(6):
            b = blk // 3
            t0 = (blk % 3) * 768
            P.ld("sp", hb, h2T[b, :, t0:t0 + 768].rearrange("(c p) t -> p c t", p=128), [], [HBB])
            P.ld("sp", wc, wcm[blk * 6:(blk + 1) * 6].rearrange("t p e -> p t e"), [], [WCB])
            for e_ in range(32):
                w2b = w2s[n2 % 2]; W2B = W2[n2 % 2]; n2 += 1
                for hc in range(2):
                    wa = w13[n13 % 4]; WA = W13[n13 % 4]; n13 += 1
                    wb3 = w13[n13 % 4]; WB3 = W13[n13 % 4]; n13 += 1
                    P.ld("pool", wa, w1[l, e_][:, hc * 256:(hc + 1) * 256].rearrange("(c p) n -> p c n", p=128), [], [WA])
                    P.ld("pool", wb3, w3[l, e_][:, hc * 256:(hc + 1) * 256].rearrange("(c p) n -> p c n", p=128), [], [WB3])
                    if hc == 0:
                        P.ld("pool", w2b, w2[l, e_].rearrange("(c p) n -> p c n", p=128), [], [W2B])
                    for c2 in range(2):
                        c = hc * 2 + c2
                        for hf in range(2):
                            pi = nu % 2; nu += 1
                            tk = slice(hf * 384, (hf + 1) * 384)
                            p1, p3 = psb[pi * 2], psb[pi * 2 + 1]
                            for kc in range(16):
                                P.mm(p1[:, 0:384], wa[:, kc, c2 * 128:(c2 + 1) * 128], hb[:, kc, tk], kc == 0, kc == 15, [WA, HBB], [PB[pi * 2]])
                            for kc in range(16):
                                P.mm(p3[:, 0:384], wb3[:, kc, c2 * 128:(c2 + 1) * 128], hb[:, kc, tk], kc == 0, kc == 15, [WB3, HBB], [PB[pi * 2 + 1]])
                            P.act(sl[pi], p1[:, 0:384], AF.Silu, [PB[pi * 2]], [SL[pi]])
                            P.tt("dve", hid[:, c, tk], p3[:, 0:384], sl[pi], ALU.mult, [PB[pi * 2 + 1], SL[pi]], [HID[c]])
                for i in range(6):
                    for g in range(4):
                        pi = 4 + g
                        for c in range(4):
                            P.mm(psb[pi][:, :], hid[:, c, i * 128:(i + 1) * 128], w2b[:, c, g * 512:(g + 1) * 512], c == 0, c == 3, HID + [W2B], [PB[pi]])
                        cs_ = slice(g * 512, (g + 1) * 512)
                        if e_ == 0:
                            P.ts("dve", acc[:, i, cs_], psb[pi][:, :], wc[:, i, e_:e_ + 1], None, ALU.mult, None, [PB[pi], WCB], [ACC[i]])
                        else:
                            P.stt(acc[:, i, cs_], psb[pi][:, :], wc[:, i, e_:e_ + 1], acc[:, i, cs_], ALU.mult, ALU.add, [PB[pi], WCB], [ACC[i]])
            for i in range(6):
                tt_ = blk * 6 + i
                w = who_of(tt_)
                x_ = xt[nx % 2]; X_ = XT[nx % 2]; nx += 1
                XOB = Buf()
                P.ld("sp", x_, xout[b, t0 + i * 128:t0 + (i + 1) * 128, :], [XOB], [X_])
                P.tt(PEW, acc[:, i, :], acc[:, i, :], gtb[:, w, :], ALU.mult, [ACC[i], GTB], [ACC[i]])
                P.tt(PEW, x_, x_, acc[:, i, :], ALU.add, [X_, ACC[i]], [X_])
                P.ld("sp", xout[b, t0 + i * 128:t0 + (i + 1) * 128, :], x_, [X_], [XOB])
        P.fence()

    toks = P.all_tokens()
    waits = P._waits("sp", (), (), toks)
    P.ops["sp"].append((waits, None, None, 0))
    P.emit()
    return nc


def norm_mod_T(P, x_, X_, s_, S_, psb, PB, ident, CB, Scol, Bcol, MB, out, OB, scratch, SCR, banks=(4, 5, 6, 7)):
    P.tt("dve", scratch, x_, x_, ALU.mult, [X_], [SCR])
    P.op("dve", lambda e: e.reduce_sum(out=s_[:, 0:1], in_=scratch, axis=mybir.AxisListType.X), [SCR], [S_])
    P.act(s_[:, 1:2], s_[:, 0:1], AF.Sqrt, [S_], [S_], scale=1.0 / D, bias=G['eps'])
    P.recip(s_[:, 2:3], s_[:, 1:2], [S_], [S_])
    P.ts("dve", x_, x_, s_[:, 2:3], None, ALU.mult, None, [X_, S_], [X_])
    for q4 in range(4):
        bk = banks[q4]
        for k in range(4):
            kc = q4 * 4 + k
            P.tr(psb[bk][:, k * 128:(k + 1) * 128], x_[:, kc * 128:(kc + 1) * 128], ident, [X_, CB], [PB[bk]])
        for k in range(4):
            kc = q4 * 4 + k
            P.act(out[:, kc, :], psb[bk][:, k * 128:(k + 1) * 128], AF.Identity, [PB[bk], MB], [OB],
                  scale=Scol[:, kc:kc + 1], bias=Bcol[:, kc:kc + 1])


def route(P, logits_ps, LPB, br_t, WRB, rt, RT):
    lg = rt[:, 32:68]
    P.tt("dve", lg, logits_ps, br_t, ALU.add, [LPB, WRB], [RT])
    gm = rt[:, 68:69]; gs = rt[:, 69:70]; m1 = rt[:, 70:71]; m2 = rt[:, 71:72]
    sel = rt[:, 72:76]; ex = rt[:, 76:80]
    P.op("dve", lambda e: e.reduce_max(out=gm, in_=lg[:, 0:4], axis=mybir.AxisListType.X), [RT], [RT])
    P.ts("dve", sel, lg[:, 0:4], gm, None, ALU.is_ge, None, [RT], [RT])
    P.ts("dve", ex, lg[:, 0:4], gm, None, ALU.subtract, None, [RT], [RT])
    P.act(ex, ex, AF.Exp, [RT], [RT])
    P.op("dve", lambda e: e.reduce_sum(out=gs, in_=ex, axis=mybir.AxisListType.X), [RT], [RT])
    P.recip(gs, gs, [RT], [RT])
    P.ts("dve", sel, sel, 1.0, 1e30, ALU.subtract, ALU.mult, [RT], [RT])
    me = rt[:, 0:32]
    P.tt("dve", me.rearrange("p (g e) -> p g e", g=4), lg[:, 4:36].rearrange("p (g e) -> p g e", g=4),
         sel.unsqueeze(2).to_broadcast([128, 4, 8]), ALU.add, [RT], [RT])
    P.op("dve", lambda e: e.reduce_max(out=m1, in_=me, axis=mybir.AxisListType.X), [RT], [RT])
    s1 = lg[:, 4:36]
    P.ts("dve", s1, me, m1, None, ALU.is_ge, None, [RT], [RT])
    P.stt(me, s1, -1e30, me, ALU.mult, ALU.add, [RT], [RT])
    P.op("dve", lambda e: e.reduce_max(out=m2, in_=me, axis=mybir.AxisListType.X), [RT], [RT])
    s2 = rt[:, 0:32]
    P.tt("dve", ex[:, 0:1], m2, m1, ALU.subtract, [RT], [RT])
    P.act(ex[:, 0:1], ex[:, 0:1], AF.Exp, [RT], [RT])
    P.ts("dve", ex[:, 1:2], ex[:, 0:1], 1.0, None, ALU.add, None, [RT], [RT])
    P.recip(ex[:, 1:2], ex[:, 1:2], [RT], [RT])
    P.tt("dve", ex[:, 2:3], ex[:, 0:1], ex[:, 1:2], ALU.mult, [RT], [RT])
    P.tt("dve", ex[:, 1:2], ex[:, 1:2], gs, ALU.mult, [RT], [RT])
    P.tt("dve", ex[:, 2:3], ex[:, 2:3], gs, ALU.mult, [RT], [RT])
    P.ts("dve", s2, me, m2, ex[:, 2:3], ALU.is_ge, ALU.mult, [RT], [RT])
    P.stt(s2, s1, ex[:, 1:2], s2, ALU.mult, ALU.add, [RT], [RT])


def _host_consts():
    ident = np.eye(128, dtype=np.float32)
    rot = np.zeros((128, 128), np.float32)
    for m in range(128):
        k = m + 16 if (m % 32) < 16 else m - 16
        rot[k, m] = 1.0
    blk = np.zeros((128, 128), np.float32)
    blk[:64, :64] = 1.0
    blk[64:, 64:] = 1.0
    cst = np.concatenate([ident, rot, blk], axis=1)
    p = np.arange(128)
    d = p % 64
    half = d // 32
    f = d % 16
    sign = np.where((d % 32) < 16, -1.0, 1.0)
    inv = (10000.0 ** (-np.arange(16, dtype=np.float32) / 16)).astype(np.float32)
    s = np.arange(SEQ)
    pos = np.stack([s // 64, s % 64], 0).astype(np.float32)
    ang = pos[half][:, :] * inv[f][:, None]
    rope = np.zeros((2, 128, T), np.float32)
    rope[0, :, :CTX] = 1.0
    rope[0, :, CTX:] = np.cos(ang.astype(np.float32))
    rope[1, :, CTX:] = np.sin(ang.astype(np.float32)) * sign[:, None]
    return cst.astype(np.float32), rope.astype(np.float32)


def _na_bias_index():
    dr = np.zeros((5, 640, 128), np.int64)
    dc = np.zeros((5, 640, 128), np.int64)
    ok = np.zeros((5, 640, 128), bool)
    for ty, j in enumerate((0, 1, 2, 14, 15)):
        ks = min(max(2 * j - 4, 0), 22)
        qi = np.arange(128)
        r = 2 * j + qi // 64
        c = qi % 64
        rs = np.clip(r - 4, 0, 24)
        qwin = np.clip(c - 8, 0, 48)
        ki = np.arange(640)
        kr = ks + ki // 64
        kc = ki % 64
        v = (kr[:, None] >= rs[None, :]) & (kr[:, None] < rs[None, :] + 8) & \
            (kc[:, None] >= qwin[None, :]) & (kc[:, None] < qwin[None, :] + 16)
        ok[ty] = v
        dr[ty] = np.clip(kr[:, None] - r[None, :] + 7, 0, 14)
        dc[ty] = np.clip(kc[:, None] - c[None, :], -15, 15) + 15
    return dr, dc, ok


def _prep_shared(inp):
    L_ = np.asarray(inp['w_mod']).shape[0]
    f = lambda a: np.ascontiguousarray(np.asarray(a, dtype=np.float32))
    sh = {}
    for k in ("w_mod", "b_mod", "w_in", "w_branch", "w_out", "w1", "w3", "w2"):
        sh[k] = f(inp[k])
    g1 = f(inp["norm1_g"]).reshape(L_, 16, 128).transpose(0, 2, 1)
    g2 = f(inp["norm2_g"]).reshape(L_, 16, 128).transpose(0, 2, 1)
    sh["gT"] = f(np.stack([g1, g2], 1))
    pv = np.zeros((L_, 128, NPV), np.float32)
    pv[:, :, 0] = inp["na_q_g"]
    pv[:, :, 1] = inp["na_k_g"]
    pv[:, :, 2] = np.concatenate([inp["df_q_g"], inp["df_q_g"]], 1)
    pv[:, :, 3] = np.concatenate([inp["df_k_g"], inp["df_k_g"]], 1)
    pv[:, :, 4] = inp["df_sub_g"]
    pv[:, :, 8:40] = np.asarray(inp["rg_conv_w"]).reshape(L_, 4, 8, 128).transpose(0, 3, 1, 2).reshape(L_, 128, 32)
    pv[:, :, 40:48] = np.asarray(inp["rg_conv_b"]).reshape(L_, 8, 128).transpose(0, 2, 1)
    pv[:, :, 48:64] = np.asarray(inp["rg_b_a"]).reshape(L_, 2, 8, 128).transpose(0, 3, 1, 2).reshape(L_, 128, 16)
    pv[:, :, 64:80] = np.asarray(inp["rg_b_x"]).reshape(L_, 2, 8, 128).transpose(0, 3, 1, 2).reshape(L_, 128, 16)
    pv[:, :, 80:96] = np.asarray(inp["rg_lam"]).reshape(L_, 2, 8, 128).transpose(0, 3, 1, 2).reshape(L_, 128, 16)
    sh["pv"] = pv
    sh["df_lam"] = f(inp["df_lam"]).reshape(L_, 256)
    dr, dc, ok = _na_bias_index()
    rpb = f(inp["na_rpb"])
    tab = np.where(ok[None, None], rpb[:, :, dr, dc], np.float32(NEGB)).astype(np.float32)
    tab = tab.reshape(L_, 4, 5, 5, 128, 128).transpose(0, 1, 4, 2, 3, 5)
    sh["nab"] = f(tab.reshape(L_, 4, 128, 5 * 5 * 128))
    wa = f(inp["rg_w_a"]); wx = f(inp["rg_w_x"])
    rg = np.stack([wa, wx], 2)
    sh["rgw"] = f(rg.transpose(0, 3, 4, 1, 2, 5).reshape(L_, 8, 128, 4 * 128))
    wr = np.concatenate([f(inp["w_router_g"]), f(inp["w_router_e"])], 2)
    sh["wr"] = f(wr.reshape(L_, 16, 128, 36).transpose(0, 2, 1, 3).reshape(L_, 128, 16 * 36))
    sh["br"] = f(np.concatenate([f(inp["b_router_g"]), f(inp["b_router_e"])], 1))
    lc = np.zeros((L_, 128, 2), np.float32)
    for l in range(L_):
        li = 0.8 - 0.6 * float(np.exp(-0.3 * l))
        lc[l, :, 0] = -li
        lc[l, :, 1] = 1.0 - li
    sh["lcst"] = lc
    cst, rope = _host_consts()
    sh["cst"] = cst
    sh["rope"] = rope
    return sh


def _core_inputs(inp, sh, core):
    b0 = core * NB
    xin = np.concatenate([np.asarray(inp["ctx"][b0:b0 + NB], np.float32), np.asarray(inp["x"][b0:b0 + NB], np.float32)], axis=1)
    cv = np.stack([np.asarray(inp["c"][b0]), np.asarray(inp["c"][b0 + 1]), np.asarray(inp["c_ctx"])], 0).astype(np.float32)
    cvT = np.ascontiguousarray(cv.reshape(3, 16, 128).transpose(2, 1, 0))
    m = dict(sh)
    m["xin"] = np.ascontiguousarray(xin)
    m["cvecT"] = cvT
    return m


LAYERED = ("w_mod", "b_mod", "w_in", "w_branch", "w_out", "w1", "w3", "w2", "gT", "pv", "df_lam", "nab",
           "rgw", "wr", "br", "lcst")


def kernel(**inputs):
    n_cores = 8
    sh = _prep_shared(inputs)
    n_l = sh["w_mod"].shape[0]
    if DEBUG.get("fused", True):
        nc = build_program(n_l, 99, ())
        in_maps = [_core_inputs(inputs, sh, c) for c in range(n_cores)]
        res = run_bass_kernel_spmd(nc, in_maps, core_ids=list(range(n_cores)))
        outs = [np.asarray(r["xout"])[:, CTX:, :] for r in res.results]
        return np.ascontiguousarray(np.concatenate(outs, axis=0).astype(np.float32))
    nc = build_program(1, 99, (), LW=1)
    base = [_core_inputs(inputs, {}, c) for c in range(n_cores)]
    xs = [m["xin"] for m in base]
    for l in range(n_l):
        in_maps = []
        for c in range(n_cores):
            m = {k: np.ascontiguousarray(sh[k][l:l + 1]) for k in LAYERED}
            m["cst"] = sh["cst"]
            m["rope"] = sh["rope"]
            m["xin"] = xs[c]
            m["cvecT"] = base[c]["cvecT"]
            in_maps.append(m)
        res = run_bass_kernel_spmd(nc, in_maps, core_ids=list(range(n_cores)))
        xs = [np.ascontiguousarray(np.asarray(r["xout"])) for r in res.results]
    outs = [x[:, CTX:, :] for x in xs]
    return np.ascontiguousarray(np.concatenate(outs, axis=0).astype(np.float32))
```
